# Optimizing a Trainium2 kernel written in Bass

```python
import math
import jax
import jax.numpy as jnp
from jax import lax
import numpy as np

D_MODEL = 2048
BATCH = 8
SEQ = 2048
DEPTH = 4

A_HEADS = 8
A_HEAD_DIM = 128
A_WIDTH = A_HEADS * A_HEAD_DIM
CHUNK = 128
B_HEADS = 8
B_HEAD_DIM = 128
B_WIDTH = B_HEADS * B_HEAD_DIM
Q_BLOCK = 128
IN_WIDTH = 2 * A_WIDTH + 3 * B_WIDTH + 2 * D_MODEL
N_EXPERTS = 16
N_GROUPS = 4
EXPERTS_PER_GROUP = N_EXPERTS // N_GROUPS
TOP_K = 2
GROUP_SCORE_K = 2
D_EXPERT = 1024
EXPERT_BLOCK = 128
PLE_DIM = 256
ALPHA = (2 * DEPTH) ** 0.25
DEEPNORM_BETA = (8 * DEPTH) ** -0.25
LN_EPS = 1e-5

kernel_name = "hybrid_gmlp_stickbreak_moe_deepnorm"


def _layer_norm(x, gain, bias):
    xf = x.astype(jnp.float32)
    mu = jnp.mean(xf, axis=-1, keepdims=True)
    xc = xf - mu
    var = jnp.mean(xc * xc, axis=-1, keepdims=True)
    y = xc * lax.rsqrt(var + LN_EPS) * gain.astype(jnp.float32) + bias.astype(jnp.float32)
    return y.astype(x.dtype)


def _chunked_spatial_gating(u, v, ws, bs):
    b, s, h, dh = v.shape
    nc = s // CHUNK
    vc = v.reshape(b, nc, CHUNK, h, dh)
    causal = jnp.tril(jnp.ones((CHUNK, CHUNK), dtype=ws.dtype))
    f = jnp.einsum('htp,bcphd->bcthd', ws * causal, vc)
    f = f + jnp.transpose(bs)[None, None, :, :, None]
    return u * f.reshape(b, s, h, dh)


def _stick_breaking_attention(q, k, v):
    b, s, h, dh = q.shape
    scale = dh ** -0.5
    outs = []
    for i in range(s // Q_BLOCK):
        kv_len = (i + 1) * Q_BLOCK
        qb = q[:, i * Q_BLOCK:kv_len]
        kb = k[:, :kv_len]
        vb = v[:, :kv_len]
        z = jnp.einsum('bqhd,bkhd->bhqk', qb, kb).astype(jnp.float32) * scale
        t_pos = i * Q_BLOCK + jnp.arange(Q_BLOCK)[:, None]
        s_pos = jnp.arange(kv_len)[None, :]
        causal = s_pos < t_pos
        log_1m_beta = jnp.where(causal, -jax.nn.softplus(z), 0.0)
        suffix = lax.cumsum(log_1m_beta, axis=3, reverse=True) - log_1m_beta
        log_a = jax.nn.log_sigmoid(z) + suffix
        a = jnp.where(causal, jnp.exp(log_a), 0.0)
        outs.append(jnp.einsum('bhqk,bkhd->bqhd', a.astype(vb.dtype), vb))
    return jnp.concatenate(outs, axis=1)


def _route(xf, router_w, router_bias):
    n = xf.shape[0]
    logits = (xf @ router_w).astype(jnp.float32)
    affinity = jax.nn.sigmoid(logits)
    sel = affinity + router_bias.astype(jnp.float32)
    sel_g = sel.reshape(n, N_GROUPS, EXPERTS_PER_GROUP)
    group_score = jnp.sum(lax.top_k(sel_g, GROUP_SCORE_K)[0], axis=-1)
    g_idx = jnp.argmax(group_score, axis=-1)
    within = jnp.take_along_axis(sel_g, g_idx[:, None, None], axis=1)[:, 0]
    _, local = lax.top_k(within, TOP_K)
    expert_idx = g_idx[:, None] * EXPERTS_PER_GROUP + local
    w = jnp.take_along_axis(affinity, expert_idx, axis=1)
    w = w / jnp.sum(w, axis=-1, keepdims=True)
    return expert_idx.astype(jnp.int32), w


def _moe(xf, expert_idx, gate_w, w_gate, w_up, w_down):
    n, d = xf.shape
    m = n * TOP_K
    flat_e = expert_idx.reshape(-1)
    flat_tok = jnp.repeat(jnp.arange(n, dtype=jnp.int32), TOP_K)
    flat_w = gate_w.reshape(-1)
    order = jnp.argsort(flat_e)
    se = flat_e[order]
    stok = flat_tok[order]
    sw = flat_w[order]
    counts = jnp.zeros((N_EXPERTS,), jnp.int32).at[flat_e].add(1)
    starts = jnp.cumsum(counts) - counts
    padded = (counts + EXPERT_BLOCK - 1) // EXPERT_BLOCK * EXPERT_BLOCK
    pends = jnp.cumsum(padded)
    pstarts = pends - padded
    dest = pstarts[se] + jnp.arange(m, dtype=jnp.int32) - starts[se]
    n_blocks = -(-m // EXPERT_BLOCK) + N_EXPERTS
    p_rows = n_blocks * EXPERT_BLOCK
    x_disp = jnp.zeros((p_rows, d), xf.dtype).at[dest].set(xf[stok])
    w_disp = jnp.zeros((p_rows,), sw.dtype).at[dest].set(sw)
    tok_disp = jnp.full((p_rows,), n, jnp.int32).at[dest].set(stok)
    block_start = jnp.arange(n_blocks, dtype=jnp.int32) * EXPERT_BLOCK
    block_e = jnp.minimum(jnp.searchsorted(pends, block_start, side='right'), N_EXPERTS - 1)

    def expert_block(args):
        xb, e = args
        hb = jax.nn.silu(xb @ w_gate[e]) * (xb @ w_up[e])
        return hb @ w_down[e]

    y = lax.map(expert_block, (x_disp.reshape(n_blocks, EXPERT_BLOCK, d), block_e))
    y = y.reshape(p_rows, d) * w_disp[:, None].astype(xf.dtype)
    return jax.ops.segment_sum(y, tok_disp, num_segments=n + 1)[:n]


def setup_inputs(seed: int = 0) -> dict:
    key = jax.random.key(seed)
    ks = jax.random.split(key, 24)

    def nrm(k, shape, scale):
        return jax.random.normal(k, shape, jnp.float32) * scale

    return {
        "x": nrm(ks[0], (BATCH, SEQ, D_MODEL), 1.0),
        "p": nrm(ks[1], (DEPTH, BATCH, SEQ, PLE_DIM), 1.0),
        "w_in": nrm(ks[2], (DEPTH, D_MODEL, IN_WIDTH), D_MODEL ** -0.5),
        "gmlp_ln_g": 1.0 + nrm(ks[3], (DEPTH, A_WIDTH), 0.02),
        "gmlp_ln_b": nrm(ks[4], (DEPTH, A_WIDTH), 0.02),
        "gmlp_ws": nrm(ks[5], (DEPTH, A_HEADS, CHUNK, CHUNK), CHUNK ** -0.5),
        "gmlp_bs": 1.0 + nrm(ks[6], (DEPTH, A_HEADS, CHUNK), 0.02),
        "w_out_a": nrm(ks[7], (DEPTH, A_WIDTH, D_MODEL), A_WIDTH ** -0.5),
        "w_out_b": nrm(ks[8], (DEPTH, B_WIDTH, D_MODEL), B_WIDTH ** -0.5),
        "w_o": nrm(ks[9], (DEPTH, D_MODEL, D_MODEL), D_MODEL ** -0.5 * DEEPNORM_BETA),
        "ln1_g": 1.0 + nrm(ks[10], (DEPTH, D_MODEL), 0.02),
        "ln1_b": nrm(ks[11], (DEPTH, D_MODEL), 0.02),
        "router_w": nrm(ks[12], (D_MODEL, N_EXPERTS), D_MODEL ** -0.5),
        "router_bias": nrm(ks[13], (N_EXPERTS,), 0.01),
        "exp_w_gate": nrm(ks[14], (DEPTH, N_EXPERTS, D_MODEL, D_EXPERT), D_MODEL ** -0.5),
        "exp_w_up": nrm(ks[15], (DEPTH, N_EXPERTS, D_MODEL, D_EXPERT), D_MODEL ** -0.5),
        "exp_w_down": nrm(ks[16], (DEPTH, N_EXPERTS, D_EXPERT, D_MODEL), D_EXPERT ** -0.5 * DEEPNORM_BETA),
        "ple_w_gate": nrm(ks[17], (DEPTH, D_MODEL, D_MODEL), D_MODEL ** -0.5),
        "ple_w_proj": nrm(ks[18], (DEPTH, PLE_DIM, D_MODEL), PLE_DIM ** -0.5 * DEEPNORM_BETA),
        "ln2_g": 1.0 + nrm(ks[19], (DEPTH, D_MODEL), 0.02),
        "ln2_b": nrm(ks[20], (DEPTH, D_MODEL), 0.02),
    }


def reference(x, p, w_in, gmlp_ln_g, gmlp_ln_b, gmlp_ws, gmlp_bs, w_out_a, w_out_b, w_o,
              ln1_g, ln1_b, router_w, router_bias, exp_w_gate, exp_w_up, exp_w_down,
              ple_w_gate, ple_w_proj, ln2_g, ln2_b):
    b, s, d = x.shape
    splits = [A_WIDTH, 2 * A_WIDTH, 2 * A_WIDTH + B_WIDTH, 2 * A_WIDTH + 2 * B_WIDTH,
              2 * A_WIDTH + 3 * B_WIDTH, 2 * A_WIDTH + 3 * B_WIDTH + D_MODEL]
    for i in range(DEPTH):
        h = x @ w_in[i]
        hu, hv, hq, hk, hvb, ga, gb = jnp.split(h, splits, axis=-1)
        u = jax.nn.gelu(hu)
        v = _layer_norm(jax.nn.gelu(hv), gmlp_ln_g[i], gmlp_ln_b[i])
        ya = _chunked_spatial_gating(u.reshape(b, s, A_HEADS, A_HEAD_DIM),
                                     v.reshape(b, s, A_HEADS, A_HEAD_DIM),
                                     gmlp_ws[i], gmlp_bs[i])
        ya = ya.reshape(b, s, A_WIDTH) @ w_out_a[i]
        ob = _stick_breaking_attention(hq.reshape(b, s, B_HEADS, B_HEAD_DIM),
                                       hk.reshape(b, s, B_HEADS, B_HEAD_DIM),
                                       hvb.reshape(b, s, B_HEADS, B_HEAD_DIM))
        yb = ob.reshape(b, s, B_WIDTH) @ w_out_b[i]
        mix = (jax.nn.sigmoid(ga) * ya + jax.nn.sigmoid(gb) * yb) @ w_o[i]
        x = _layer_norm(ALPHA * x + mix, ln1_g[i], ln1_b[i])
        xf = x.reshape(b * s, d)
        expert_idx, gate_w = _route(xf, router_w, router_bias)
        ffn = _moe(xf, expert_idx, gate_w, exp_w_gate[i], exp_w_up[i], exp_w_down[i]).reshape(b, s, d)
        ple = jax.nn.sigmoid(x @ ple_w_gate[i]) * (p[i] @ ple_w_proj[i])
        x = _layer_norm(ALPHA * x + ffn + ple, ln2_g[i], ln2_b[i])
    return x
```

```python
import numpy as np
import concourse.bass as bass
import concourse.mybir as mybir
from concourse.bass_utils import run_bass_kernel_spmd

F32 = mybir.dt.float32
BF16 = mybir.dt.bfloat16
AF = mybir.ActivationFunctionType
ALU = mybir.AluOpType
AX = mybir.AxisListType

S = 2048
D = 2048
KC = 16
INW = 9216
NE = 16
DE = 1024
DEPTH = 4
ALPHA = float((2 * DEPTH) ** 0.25)
EPS = 1e-5
SCALE = float(128 ** -0.5)
NCORES = 8
EPC = 1
NCONST = 128 * 4 + 4 * 512


class Prog:
    def __init__(self, esem, dsems):
        self.q = {n: [] for n in ("pe", "dve", "act", "pool", "sp")}
        self.seen = {n: {} for n in self.q}
        self.esem = esem
        self.ecount = {}
        self.epoch = 0
        self.dsems = dsems
        self.dcount = {}
        self.drr = {n: 0 for n in dsems}
        self.lastw = {}
        self.readers = {}

    def semh(self, k):
        if isinstance(k, tuple):
            return self.dsems[k[0]][k[1]]
        return self.esem[k]

    def op(self, eng, fn, reads=(), writes=(), dma=False):
        seen = self.seen[eng]
        waits = []

        def need(dep):
            if dep is None:
                return
            k, v = dep
            if eng == "pe" and isinstance(k, str) and k.startswith("pe#"):
                return
            if seen.get(k, 0) >= v:
                return
            seen[k] = v
            waits.append((k, v))

        for r in reads:
            need(self.lastw.get(r))
        for w in writes:
            need(self.lastw.get(w))
            for k, v in self.readers.get(w, {}).items():
                need((k, v))
        if dma:
            lst = self.dsems[eng]
            i = self.drr[eng]
            self.drr[eng] = (i + 1) % len(lst)
            k = (eng, i)
            prev = self.dcount.get(k, 0)
            if prev:
                need((k, prev))
            self.dcount[k] = prev + 16
            comp = (k, prev + 16)
        else:
            ek = eng + "#" + str(self.epoch)
            self.ecount[ek] = self.ecount.get(ek, 0) + 1
            comp = (ek, self.ecount[ek])
        for r in reads:
            d = self.readers.setdefault(r, {})
            d[comp[0]] = max(d.get(comp[0], 0), comp[1])
        for w in writes:
            self.lastw[w] = comp
            self.readers[w] = {}
        self.q[eng].append((waits, fn, comp))

    def barrier(self):
        allk = [(k, v) for k, v in self.ecount.items() if v > 0]
        allk += [(k, v) for k, v in self.dcount.items() if v > 0]
        for eng in self.q:
            seen = self.seen[eng]
            waits = []
            for k, v in allk:
                if isinstance(k, str) and k.split("#")[0] == eng:
                    continue
                if seen.get(k, 0) >= v:
                    continue
                seen[k] = v
                waits.append((k, v))
            if waits:
                self.q[eng].append((waits, None, None))
        self.lastw.clear()
        self.readers.clear()

    def run(self, eng, e):
        for waits, fn, comp in self.q[eng]:
            for k, v in waits:
                e.wait_ge(self.semh(k), v)
            if fn is None:
                continue
            ins = fn(e)
            k, v = comp
            ins.then_inc(self.semh(k), 16 if isinstance(k, tuple) else 1)


def build(epc, depth_run, wd=DEPTH):
    nc = bass.Bass("TRN2", target_bir_lowering=False)
    dt = nc.dram_tensor

    def ein(name, shape):
        return dt(name, shape, F32, kind="ExternalInput").ap()

    x_in = ein("x", [epc * S, D])
    xT_in = ein("xT", [epc * D, S])
    pT_in = ein("pT", [wd * epc * 256, S])
    w_in = ein("w_in", [wd * D, INW])
    w_oa = ein("w_out_a", [wd * 1024, D])
    w_ob = ein("w_out_b", [wd * 1024, D])
    w_o = ein("w_o", [wd * D, D])
    eg = ein("exp_g", [wd * NE * D, DE])
    eu = ein("exp_u", [wd * NE * D, DE])
    ed = ein("exp_d", [wd * NE * DE, D])
    plg = ein("ple_g", [wd * D, D])
    plp = ein("ple_p", [wd * 256, D])
    rw = ein("router_w", [D, 16])
    rb = ein("router_bias", [1, 16])
    gg = ein("gmlp_ln_g", [wd, 1024])
    gb = ein("gmlp_ln_b", [wd, 1024])
    l1g = ein("ln1_g", [wd, D])
    l1b = ein("ln1_b", [wd, D])
    l2g = ein("ln2_g", [wd, D])
    l2b = ein("ln2_b", [wd, D])
    bsd = ein("gmlp_bs", [wd, 1024])
    wsT = ein("gmlp_wsT", [wd * 8 * 128, 128])
    cst = ein("consts", [128, NCONST])
    out = dt("out", [epc * S, D], F32, kind="ExternalOutput").ap()
    xa = dt("xa", [S, D], F32).ap()
    xb = dt("xb", [S, D], F32).ap()
    xaT = dt("xaT", [D, S], BF16).ap()
    x1T = dt("x1T", [D, S], BF16).ap()
    mixT = dt("mixT", [D, S], BF16).ap()
    cwT_d = dt("cwT_d", [16, S], F32).ap()

    import contextlib
    with contextlib.ExitStack() as st:
        R1t = st.enter_context(nc.sbuf_tensor("R1", [128, 32768], BF16))
        R2t = st.enter_context(nc.sbuf_tensor("R2", [128, 32768], BF16))
        ARt = st.enter_context(nc.sbuf_tensor("AR", [128, 15360], F32))
        CSTt = st.enter_context(nc.sbuf_tensor("CST", [128, NCONST], F32))
        CSTBt = st.enter_context(nc.sbuf_tensor("CSTB", [128, 256], BF16))
        PS = [st.enter_context(nc.psum_tensor(f"ps{i}", [128, 512], F32)) for i in range(8)]
        esem = {f"{n}#{ep}": st.enter_context(nc.semaphore(f"s_{n}_{ep}")) for n in ("pe", "dve", "act", "pool") for ep in range(epc)}
        dsems = {
            "sp": [st.enter_context(nc.semaphore(f"d_sp{i}")) for i in range(8)],
            "pool": [st.enter_context(nc.semaphore(f"d_pl{i}")) for i in range(8)],
        }
        block = st.enter_context(nc.Block())
        pg = Prog(esem, dsems)

        R1f = R1t[:]
        R2f = R2t[:]
        XT = R1f.rearrange("p (k s) -> p k s", k=16)
        R2v = R2f.rearrange("p (k s) -> p k s", k=16)
        IDENT = CSTt[:, 0:128]
        TRI = CSTt[:, 128:256]
        ONES = CSTt[:, 256:384]
        TRIU = CSTt[:, 384:512]
        MASKD = [CSTt[:, 512 + j * 512:512 + (j + 1) * 512] for j in range(4)]

        class Arena:
            def __init__(self):
                self.off = 0

            def reset(self):
                self.off = 0

            def f32(self, n):
                a = ARt[:, self.off:self.off + n]
                self.off += n
                assert self.off <= 15360, self.off
                return a

            def bf16(self, n):
                assert n % 2 == 0
                a = ARt[:, self.off:self.off + n // 2].bitcast(BF16)
                self.off += n // 2
                assert self.off <= 15360, self.off
                return a

        ar = Arena()
        uid = [0]

        def nid(tag):
            uid[0] += 1
            return (tag, uid[0])

        def mm(groups, reads, wid):
            def fn(e, groups=groups):
                ins = None
                for o, pairs in groups:
                    n = len(pairs)
                    for i, (l, r) in enumerate(pairs):
                        ins = e.matmul(o, l, r, start=(i == 0), stop=(i == n - 1))
                return ins
            pg.op("pe", fn, reads=reads, writes=[wid])

        def tr(groups, reads, wid):
            def fn(e, groups=groups):
                ins = None
                for o, i_ in groups:
                    ins = e.transpose(o, i_, IDENT)
                return ins
            pg.op("pe", fn, reads=reads, writes=[wid])

        def act(o, i_, func, reads, writes, scale=1.0, bias=0.0):
            def fn(e, o=o, i_=i_):
                return e.activation(out=o, in_=i_, func=func, bias=bias, scale=scale)
            pg.op("act", fn, reads=reads, writes=writes)

        def tt(o, a, b, op, reads, writes, eng="dve"):
            def fn(e, o=o, a=a, b=b):
                return e.tensor_tensor(out=o, in0=a, in1=b, op=op)
            pg.op(eng, fn, reads=reads, writes=writes)

        def ts(o, a, s1, s2, op0, op1, reads, writes):
            def fn(e, o=o, a=a):
                if s2 is None:
                    return e.tensor_scalar(out=o, in0=a, scalar1=s1, scalar2=None, op0=op0)
                return e.tensor_scalar(out=o, in0=a, scalar1=s1, scalar2=s2, op0=op0, op1=op1)
            pg.op("dve", fn, reads=reads, writes=writes)

        def stt(o, a, sc, b, op0, op1, reads, writes):
            def fn(e, o=o, a=a, b=b):
                return e.scalar_tensor_tensor(out=o, in0=a, scalar=sc, in1=b, op0=op0, op1=op1)
            pg.op("dve", fn, reads=reads, writes=writes)

        def cp(o, i_, reads, writes, eng="dve"):
            if eng == "act":
                def fn(e, o=o, i_=i_):
                    return e.copy(out=o, in_=i_)
            else:
                def fn(e, o=o, i_=i_):
                    return e.tensor_copy(out=o, in_=i_)
            pg.op(eng, fn, reads=reads, writes=writes)

        def red(o, i_, op, reads, writes):
            def fn(e, o=o, i_=i_):
                return e.tensor_reduce(out=o, in_=i_, axis=AX.X, op=op)
            pg.op("dve", fn, reads=reads, writes=writes)

        def dma(o, i_, reads, writes, cast=False):
            eng = "pool" if cast else "sp"

            def fn(e, o=o, i_=i_):
                return e.dma_start(out=o, in_=i_)
            pg.op(eng, fn, reads=reads, writes=writes, dma=True)

        def bcast_load(dst, src_row, wid):
            dma(dst, src_row.partition_broadcast(128)[:, 0, :], [], [wid])

        def kview(ap2d, kc):
            return ap2d.rearrange("(k p) n -> p k n", p=128)

        psr = [0]

        def bank(lo=0, hi=8):
            b = lo + psr[0] % (hi - lo)
            psr[0] += 1
            return b

        def gelu_a(ps_ap, psid, T1, t1id):
            act(T1, ps_ap, AF.Square, [psid], [t1id])
            ts(T1, T1, 0.044715, 1.0, ALU.mult, ALU.add, [t1id], [t1id])
            tt(T1, T1, ps_ap, ALU.mult, [t1id, psid], [t1id])

        def gelu_b(ps_ap, psid, dst, dstid, T1, T2, t1id, t2id):
            act(T2, T1, AF.Sigmoid, [t1id], [t2id], scale=1.5957691216057308)
            tt(dst, T2, ps_ap, ALU.mult, [t2id, psid], [dstid])

        def gelu(ps_ap, psid, dst, dstid, T1, T2, t1id, t2id):
            gelu_a(ps_ap, psid, T1, t1id)
            gelu_b(ps_ap, psid, dst, dstid, T1, T2, t1id, t2id)

        def layer_norm(T, tid, G, B, gid, bid, n, ST, MV, RS, smid):
            nch = n // 512
            for c in range(nch):
                def fn(e, c=c):
                    return e.bn_stats(out=ST[:, c * 6:(c + 1) * 6], in_=T[:, c * 512:(c + 1) * 512])
                pg.op("dve", fn, reads=[tid], writes=[smid])
            def fn2(e):
                return e.bn_aggr(out=MV, in_=ST[:, 0:nch * 6])
            pg.op("dve", fn2, reads=[smid], writes=[smid])
            act(RS, MV[:, 1:2], AF.Sqrt, [smid], ["RSQ"], bias=EPS)
            def fn3(e):
                return e.reciprocal(out=RS, in_=RS)
            pg.op("dve", fn3, reads=["RSQ"], writes=[smid])
            ts(MV[:, 1:2], MV[:, 0:1], RS, -1.0, ALU.mult, ALU.mult, [smid], [smid])
            def fn4(e):
                return e.activation(out=T, in_=T, func=AF.Identity, bias=MV[:, 1:2], scale=RS)
            pg.op("act", fn4, reads=[tid, smid], writes=[tid])
            tt(T, T, G, ALU.mult, [tid, gid], [tid], eng="pool")

        dma(CSTt[:], cst[:, :], [], ["CST"])
        cp(CSTBt[:, 0:256], CSTt[:, 128:384], ["CST"], ["CSTB"])
        TRIB = CSTBt[:, 0:128]
        ONESB = CSTBt[:, 128:256]
        pg.barrier()

        for e_i in range(epc):
            pg.epoch = e_i
            for l in range(depth_run):
                last = (l == depth_run - 1)
                wl = w_in[l * D:(l + 1) * D, :]
                for kc in range(KC):
                    if l == 0:
                        dma(XT[:, kc, :], xT_in[e_i * D + kc * 128:e_i * D + (kc + 1) * 128, :], [], [("XT", kc)], cast=True)
                    else:
                        dma(XT[:, kc, :], xaT[kc * 128:(kc + 1) * 128, :], [], [("XT", kc)])
                xt_ids = [("XT", kc) for kc in range(KC)]
                ar.reset()
                WS = [ar.bf16(16 * 512).rearrange("p (k n) -> p k n", k=16) for _ in range(2)]
                TT = [[ar.f32(512), ar.f32(512)] for _ in range(2)]
                for grp in range(2):
                    dma(WS[grp], kview(wl[:, grp * 512:(grp + 1) * 512], 16), [], [("WS", grp)], cast=True)
                pend = None
                cnt = 0
                for grp in range(2):
                    wsid = ("WS", grp)
                    for cc in range(4):
                        h = grp * 4 + cc
                        for tg in range(4):
                            b = bank()
                            mm([(PS[b][:], [(WS[grp][:, kc, cc * 128:(cc + 1) * 128], XT[:, kc, tg * 512:(tg + 1) * 512]) for kc in range(KC)])],
                               [wsid] + xt_ids, ("ps", b))
                            s_ = cnt % 2
                            cnt += 1
                            gelu_a(PS[b][:], ("ps", b), TT[s_][0], ("T1", s_))
                            if pend is not None:
                                gelu_b(*pend)
                            pend = (PS[b][:], ("ps", b), R2v[:, h, tg * 512:(tg + 1) * 512], ("R2", h, tg),
                                    TT[s_][0], TT[s_][1], ("T1", s_), ("T2", s_))
                gelu_b(*pend)
                pg.barrier()
                ar.reset()
                VW = R2f[:, 16384:32768].rearrange("p (k n) -> p k n", k=16)
                for hf in range(2):
                    dma(VW[:, :, hf * 512:(hf + 1) * 512], kview(wl[:, 1024 + hf * 512:1024 + (hf + 1) * 512], 16), [], [("VW", hf)], cast=True)
                GG = ar.f32(1024)
                GB_ = ar.f32(1024)
                BSB = ar.f32(1024)
                WSF = ar.f32(1024)
                WSB = ar.bf16(1024)
                VG = [ar.f32(1024) for _ in range(2)]
                VC = [ar.bf16(1024) for _ in range(3)]
                T1s = [[ar.f32(512), ar.f32(512)] for _ in range(2)]
                FB = [ar.f32(512) for _ in range(2)]
                ST = ar.f32(24)
                MV = ar.f32(2)
                RS = ar.f32(1)
                bcast_load(GG, gg[l:l + 1, :], "GG")
                bcast_load(GB_, gb[l:l + 1, :], "GB")
                bcast_load(BSB, bsd[l:l + 1, :], "BSB")
                dma(WSF.rearrange("p (h t) -> p h t", h=8), wsT[l * 1024:(l + 1) * 1024, :].rearrange("(h p) t -> p h t", p=128), [], ["WSF"])
                for h in range(8):
                    tt(WSB[:, h * 128:(h + 1) * 128], WSF[:, h * 128:(h + 1) * 128], TRIU, ALU.mult, ["WSF", "CST"], [("WSB", h)])
                wsb_ids = [("WSB", h) for h in range(8)]
                cnt = [0]

                def s1b_A(c):
                    s_ = c % 2
                    vgid = ("VG", s_)
                    for hf in range(2):
                        b = bank()
                        mm([(PS[b][:], [(XT[:, kc, c * 128:(c + 1) * 128], VW[:, kc, hf * 512:(hf + 1) * 512]) for kc in range(KC)])],
                           [("VW", hf)] + xt_ids, ("ps", b))
                        t_ = cnt[0] % 2
                        cnt[0] += 1
                        gelu(PS[b][:], ("ps", b), VG[s_][:, hf * 512:(hf + 1) * 512], vgid,
                             T1s[t_][0], T1s[t_][1], ("T1", t_), ("T2", t_))
                    layer_norm(VG[s_], vgid, GG, GB_, "GG", "GB", 1024, ST, MV, RS, "SM")
                    tt(VC[c % 3], VG[s_], GB_, ALU.add, [vgid, "GB"], [("VC", c % 3)], eng="pool")

                def s1b_B(c):
                    s_ = c % 3
                    for hh in range(2):
                        b = bank()
                        mm([(PS[b][:, j * 128:(j + 1) * 128], [(VC[s_][:, (hh * 4 + j) * 128:(hh * 4 + j + 1) * 128], WSB[:, (hh * 4 + j) * 128:(hh * 4 + j + 1) * 128])]) for j in range(4)],
                           [("VC", s_)] + wsb_ids, ("ps", b))
                        f_ = hh
                        F1 = FB[f_]
                        tt(F1, PS[b][:], BSB[:, hh * 512:(hh + 1) * 512], ALU.add, [("ps", b), "BSB"], [("F1", f_)])
                        dst = R2v[:, hh * 4:(hh + 1) * 4, c * 128:(c + 1) * 128]
                        ids = [("R2", hh * 4 + j, c // 4) for j in range(4)]
                        def fn(e, dst=dst, F1=F1):
                            return e.tensor_tensor(out=dst, in0=F1.rearrange("p (h t) -> p h t", h=4), in1=dst, op=ALU.mult)
                        pg.op("dve", fn, reads=[("F1", f_)] + ids, writes=ids)

                for it in range(18):
                    if it < 16:
                        s1b_A(it)
                    if it >= 2:
                        s1b_B(it - 2)
                pg.barrier()
                ar.reset()
                WQ = ar.bf16(16 * 128).rearrange("p (k n) -> p k n", k=16)
                WK = ar.bf16(16 * 128).rearrange("p (k n) -> p k n", k=16)
                WVh = ar.bf16(16 * 128).rearrange("p (k n) -> p k n", k=16)
                QT = ar.bf16(2048)
                KT = ar.bf16(2048)
                VH = ar.bf16(2048)
                E_ = [ar.f32(512) for _ in range(2)]
                SP = [ar.bf16(512) for _ in range(3)]
                LA = [ar.f32(512) for _ in range(2)]
                A_ = [ar.bf16(512) for _ in range(3)]
                for h in range(8):
                    dma(WQ, kview(wl[:, 2048 + h * 128:2048 + (h + 1) * 128], 16), [], ["WQ"], cast=True)
                    dma(WK, kview(wl[:, 3072 + h * 128:3072 + (h + 1) * 128], 16), [], ["WK"], cast=True)
                    dma(WVh, kview(wl[:, 4096 + h * 128:4096 + (h + 1) * 128], 16), [], ["WV"], cast=True)
                    for tg in range(4):
                        b = bank(0, 2)
                        mm([(PS[b][:], [(WQ[:, kc, :], XT[:, kc, tg * 512:(tg + 1) * 512]) for kc in range(KC)])], ["WQ"] + xt_ids, ("ps", b))
                        cp(QT[:, tg * 512:(tg + 1) * 512], PS[b][:], [("ps", b)], [("QT", tg)], eng="act")
                        b = bank(0, 2)
                        mm([(PS[b][:], [(WK[:, kc, :], XT[:, kc, tg * 512:(tg + 1) * 512]) for kc in range(KC)])], ["WK"] + xt_ids, ("ps", b))
                        cp(KT[:, tg * 512:(tg + 1) * 512], PS[b][:], [("ps", b)], [("KT", tg)])
                        b = bank(0, 2)
                        mm([(PS[b][:, j * 128:(j + 1) * 128], [(XT[:, kc, (tg * 4 + j) * 128:(tg * 4 + j + 1) * 128], WVh[:, kc, :]) for kc in range(KC)]) for j in range(4)],
                           ["WV"] + xt_ids, ("ps", b))
                        cp(VH[:, tg * 512:(tg + 1) * 512], PS[b][:], [("ps", b)], [("VH", tg)], eng="act")
                    for g in range(4):
                        t0 = g * 512
                        ob = 6 + g % 2
                        tiles = list(range(4 * g + 3, -1, -1))
                        n = len(tiles)

                        def stZ(i, g=g, t0=t0, tiles=tiles):
                            kc = tiles[i]
                            zb = 1 + i % 3
                            mm([(PS[zb][:], [(KT[:, kc * 128:(kc + 1) * 128], QT[:, t0:t0 + 512])])], [("KT", kc // 4), ("QT", g)], ("ps", zb))

                        def stA(i, g=g, tiles=tiles):
                            kc = tiles[i]
                            zb = 1 + i % 3
                            e_, sp_ = i % 2, i % 3
                            diag = kc >= 4 * g
                            j = kc - 4 * g
                            act(E_[e_], PS[zb][:], AF.Exp, [("ps", zb)], [("E", e_)], scale=SCALE)
                            act(SP[sp_], E_[e_], AF.Ln, [("E", e_)], [("SP", sp_)], bias=1.0)
                            if diag:
                                tt(SP[sp_], SP[sp_], MASKD[j], ALU.mult, [("SP", sp_), "CST"], [("SP", sp_)])

                        def stB(i, g=g, tiles=tiles):
                            kc = tiles[i]
                            zb = 1 + i % 3
                            sb = 4 + i % 2
                            sp_, la_, a_ = i % 3, i % 2, i % 3
                            diag = kc >= 4 * g
                            j = kc - 4 * g
                            rb = 0
                            if i > 0:
                                def fnr_(e, rb=rb, i=i):
                                    return e.matmul(PS[rb][:], ONESB, SP[(i - 1) % 3], start=(i == 1), stop=True, skip_group_check=True)
                                pg.op("pe", fnr_, reads=[("SP", (i - 1) % 3), "CSTB"], writes=[("ps", rb)])
                            mm([(PS[sb][:], [(TRIB, SP[sp_])])], [("SP", sp_), "CSTB"], ("ps", sb))
                            stt(LA[la_], PS[zb][:], SCALE, SP[sp_], ALU.mult, ALU.subtract, [("ps", zb), ("SP", sp_)], [("LA", la_)])
                            tt(LA[la_], LA[la_], PS[sb][:], ALU.subtract, [("LA", la_), ("ps", sb)], [("LA", la_)])
                            if i > 0:
                                tt(LA[la_], LA[la_], PS[rb][:], ALU.subtract, [("LA", la_), ("ps", rb)], [("LA", la_)])
                            act(A_[a_], LA[la_], AF.Exp, [("LA", la_)], [("A", a_)])
                            if diag:
                                tt(A_[a_], A_[a_], MASKD[j], ALU.mult, [("A", a_), "CST"], [("A", a_)])

                        def stC(i, ob=ob, tiles=tiles, n=n):
                            kc = tiles[i]
                            a_ = i % 3
                            def fn(e, ob=ob, kc=kc, a_=a_, i=i, n=n):
                                return e.matmul(PS[ob][:], VH[:, kc * 128:(kc + 1) * 128], A_[a_], start=(i == 0), stop=(i == n - 1), skip_group_check=True)
                            pg.op("pe", fn, reads=[("VH", kc // 4), ("A", a_)], writes=[("ps", ob)])

                        stZ(0)
                        for it in range(n + 2):
                            if it < n:
                                stA(it)
                            if 0 <= it - 1 < n:
                                stB(it - 1)
                            if 0 <= it - 2 < n:
                                stC(it - 2)
                            if it + 1 < n:
                                stZ(it + 1)
                        cp(R2v[:, 8 + h, t0:t0 + 512], PS[ob][:], [("ps", ob)], [("R2", 8 + h, g)], eng="act")
                pg.barrier()
                ar.reset()
                WGA = [ar.bf16(16 * 256).rearrange("p (k n) -> p k n", k=16) for _ in range(2)]
                WGB = [ar.bf16(16 * 256).rearrange("p (k n) -> p k n", k=16) for _ in range(2)]
                WA = [ar.bf16(8 * 256).rearrange("p (k n) -> p k n", k=8) for _ in range(2)]
                WB = [ar.bf16(8 * 256).rearrange("p (k n) -> p k n", k=8) for _ in range(2)]
                TT = [[ar.f32(512), ar.f32(512)] for _ in range(2)]
                MST = [ar.bf16(2048) for _ in range(1)]
                cnt = 0
                for cgp in range(8):
                    w_ = cgp % 2
                    c0 = cgp * 256
                    dma(WGA[w_], kview(wl[:, 5120 + c0:5120 + c0 + 256], 16), [], [("WGA", w_)], cast=True)
                    dma(WA[w_], kview(w_oa[l * 1024:(l + 1) * 1024, c0:c0 + 256], 8), [], [("WA", w_)], cast=True)
                    dma(WGB[w_], kview(wl[:, 7168 + c0:7168 + c0 + 256], 16), [], [("WGB", w_)], cast=True)
                    dma(WB[w_], kview(w_ob[l * 1024:(l + 1) * 1024, c0:c0 + 256], 8), [], [("WB", w_)], cast=True)
                    for cc in range(2):
                        c = cgp * 2 + cc
                        m_ = 0
                        for tg in range(4):
                            tsl = slice(tg * 512, (tg + 1) * 512)
                            csl = slice(cc * 128, (cc + 1) * 128)
                            bga, bya, bgb, byb = bank(), bank(), bank(), bank()
                            mm([(PS[bga][:], [(WGA[w_][:, kc, csl], XT[:, kc, tsl]) for kc in range(16)])], [("WGA", w_)] + xt_ids, ("ps", bga))
                            mm([(PS[bya][:], [(WA[w_][:, kc, csl], R2v[:, kc, tsl]) for kc in range(8)])], [("WA", w_)] + [("R2", kc, tg) for kc in range(8)], ("ps", bya))
                            mm([(PS[bgb][:], [(WGB[w_][:, kc, csl], XT[:, kc, tsl]) for kc in range(16)])], [("WGB", w_)] + xt_ids, ("ps", bgb))
                            mm([(PS[byb][:], [(WB[w_][:, kc, csl], R2v[:, 8 + kc, tsl]) for kc in range(8)])], [("WB", w_)] + [("R2", 8 + kc, tg) for kc in range(8)], ("ps", byb))
                            s_ = cnt % 2
                            cnt += 1
                            T1, T2 = TT[s_]
                            act(T1, PS[bga][:], AF.Sigmoid, [("ps", bga)], [("T1", s_)])
                            act(T2, PS[bgb][:], AF.Sigmoid, [("ps", bgb)], [("T2", s_)])
                            tt(T1, T1, PS[bya][:], ALU.mult, [("T1", s_), ("ps", bya)], [("T1", s_)])
                            tt(T2, T2, PS[byb][:], ALU.mult, [("T2", s_), ("ps", byb)], [("T2", s_)])
                            tt(MST[m_][:, tsl], T1, T2, ALU.add, [("T1", s_), ("T2", s_)], [("MST", m_, tg)])
                        dma(mixT[c * 128:(c + 1) * 128, :], MST[m_], [("MST", m_, tg) for tg in range(4)], [("mixT", c)])
                pg.barrier()
                ar.reset()
                WO = XT
                for kc in range(KC):
                    dma(WO[:, kc, :], w_o[l * D + kc * 128:l * D + (kc + 1) * 128, :], [], [("WO", kc)], cast=True)
                    dma(R2v[:, kc, :], mixT[kc * 128:(kc + 1) * 128, :], [], [("MX", kc)])
                wo_ids = [("WO", kc) for kc in range(KC)]
                mx_ids = [("MX", kc) for kc in range(KC)]
                Tb = [ar.f32(2048) for _ in range(3)]
                G1 = ar.f32(2048)
                B1 = ar.f32(2048)
                X1F = ar.f32(2048)
                X1B = ar.bf16(2048)
                RW = ar.f32(256)
                RB = ar.f32(16)
                CWT = [ar.f32(128) for _ in range(2)]
                ST = ar.f32(24)
                MV = ar.f32(2)
                RS = ar.f32(1)
                sm = [ar.f32(16) for _ in range(8)]
                AFF, SEL, EQ, SEL2, EM, W_, CW, _u = sm
                CWs = [CW, _u]
                M1 = ar.f32(4)
                M2 = ar.f32(4)
                GS = ar.f32(4)
                GMK = ar.f32(4)
                GM = ar.f32(1)
                WSUM = ar.f32(1)
                RWS = ar.f32(1)
                bcast_load(G1, l1g[l:l + 1, :], "G1")
                bcast_load(B1, l1b[l:l + 1, :], "B1")
                bcast_load(RB, rb[0:1, :], "RB")
                dma(RW.rearrange("p (k e) -> p k e", k=16), rw.rearrange("(k p) e -> p k e", p=128), [], ["RW"])

                def s4_load(tb):
                    T = Tb[tb % 3]
                    if l == 0:
                        dma(T, x_in[e_i * S + tb * 128:e_i * S + (tb + 1) * 128, :], [], [("T", tb % 3)])
                    else:
                        dma(T, xa[tb * 128:(tb + 1) * 128, :], [], [("T", tb % 3)])

                def s4_A(tb):
                    T = Tb[tb % 3]
                    tid = ("T", tb % 3)
                    rows = slice(tb * 128, (tb + 1) * 128)
                    for cg in range(4):
                        mm([(PS[cg][:], [(R2v[:, kc, rows], WO[:, kc, cg * 512:(cg + 1) * 512]) for kc in range(KC)])], mx_ids + wo_ids, ("ps", cg))
                        stt(T[:, cg * 512:(cg + 1) * 512], T[:, cg * 512:(cg + 1) * 512], ALPHA, PS[cg][:], ALU.mult, ALU.add, [tid, ("ps", cg)], [tid])
                    layer_norm(T, tid, G1, B1, "G1", "B1", 2048, ST, MV, RS, "SM")
                    tt(T, T, B1, ALU.add, [tid, "B1"], [tid], eng="pool")
                    dma(xb[rows, :], T, [tid], [("xb", tb)])

                def s4_B(tb):
                    T = Tb[tb % 3]
                    tid = ("T", tb % 3)
                    rows = slice(tb * 128, (tb + 1) * 128)
                    for q4 in range(4):
                        b_ = 4 + q4 % 2
                        tr([(PS[b_][:, j * 128:(j + 1) * 128], T[:, (q4 * 4 + j) * 128:(q4 * 4 + j + 1) * 128]) for j in range(4)], [tid, "CST"], ("ps", b_))
                        cp(X1F[:, q4 * 512:(q4 + 1) * 512], PS[b_][:], [("ps", b_)], [("X1F", q4)], eng="act")
                        cp(X1B[:, q4 * 512:(q4 + 1) * 512], X1F[:, q4 * 512:(q4 + 1) * 512], [("X1F", q4)], [("X1B", q4)])
                    dma(x1T.rearrange("(k p) s -> p k s", p=128)[:, :, rows], X1B.rearrange("p (k t) -> p k t", k=16), [("X1B", q) for q in range(4)], [("x1T", tb)])
                    X1Fv = X1F.rearrange("p (k t) -> p k t", k=16)
                    RWv = RW.rearrange("p (k e) -> p k e", k=16)
                    mm([(PS[6][:, 0:16], [(X1Fv[:, kc, :], RWv[:, kc, :]) for kc in range(KC)])], [("X1F", q) for q in range(4)] + ["RW"], ("ps", 6))
                    act(AFF, PS[6][:, 0:16], AF.Sigmoid, [("ps", 6)], ["AFF"])
                    tt(SEL, AFF, RB, ALU.add, ["AFF", "RB"], ["SEL"])
                    red(M1, SEL.rearrange("p (g e) -> p g e", g=4), ALU.max, ["SEL"], ["M1"])
                    for g_ in range(4):
                        ts(EQ[:, g_ * 4:(g_ + 1) * 4], SEL[:, g_ * 4:(g_ + 1) * 4], M1[:, g_:g_ + 1], None, ALU.is_equal, None, ["SEL", "M1"], ["EQ"])
                    stt(SEL2, EQ, -1.0e9, SEL, ALU.mult, ALU.add, ["EQ", "SEL"], ["SEL2"])
                    red(M2, SEL2.rearrange("p (g e) -> p g e", g=4), ALU.max, ["SEL2"], ["M2"])
                    tt(GS, M1, M2, ALU.add, ["M1", "M2"], ["GS"])
                    red(GM, GS, ALU.max, ["GS"], ["GM"])
                    ts(GMK, GS, GM[:, 0:1], None, ALU.is_equal, None, ["GS", "GM"], ["GMK"])
                    for g_ in range(4):
                        ts(EM[:, g_ * 4:(g_ + 1) * 4], SEL[:, g_ * 4:(g_ + 1) * 4], M2[:, g_:g_ + 1], GMK[:, g_:g_ + 1], ALU.is_ge, ALU.mult, ["SEL", "M2", "GMK"], ["EM"])
                    tt(W_, AFF, EM, ALU.mult, ["AFF", "EM"], ["W"])
                    red(WSUM, W_, ALU.add, ["W"], ["WSUM"])
                    def fnr(e):
                        return e.reciprocal(out=RWS, in_=WSUM)
                    pg.op("dve", fnr, reads=["WSUM"], writes=["RWS"])
                    ts(CWs[tb % 2], W_, RWS[:, 0:1], None, ALU.mult, None, ["W", "RWS"], [("CW", tb % 2)])

                def s4_C(tb):
                    rows = slice(tb * 128, (tb + 1) * 128)
                    c_ = tb % 2
                    tr([(PS[7][0:16, 0:128], CWs[c_])], [("CW", c_), "CST"], ("ps", 7))
                    cp(CWT[c_][0:16, :], PS[7][0:16, 0:128], [("ps", 7)], [("CWT", c_)], eng="act")
                    dma(cwT_d[:, rows], CWT[c_][0:16, :], [("CWT", c_)], [("cwT_d", tb)])

                s4_load(0)
                for it in range(18):
                    if it + 1 < 16:
                        s4_load(it + 1)
                    if it < 16:
                        s4_A(it)
                    if 1 <= it <= 16:
                        s4_B(it - 1)
                    if 2 <= it <= 17:
                        s4_C(it - 2)
                pg.barrier()
                YACC = R1f.bitcast(F32).rearrange("p (t n) -> p t n", t=8)
                X1H = R2f[:, 0:16384].rearrange("p (k s) -> p k s", k=16)
                HS = R2f[:, 16384:24576].rearrange("p (k s) -> p k s", k=8)
                WD = [R2f[:, 24576 + i * 4096:24576 + (i + 1) * 4096].rearrange("p (k n) -> p k n", k=8) for i in range(2)]
                for hf in range(2):
                    ar.reset()
                    WG = [ar.bf16(16 * 256).rearrange("p (k n) -> p k n", k=16) for _ in range(2)]
                    WU = [ar.bf16(16 * 256).rearrange("p (k n) -> p k n", k=16) for _ in range(2)]
                    BC = [ar.f32(1024) for _ in range(2)]
                    TT = [[ar.f32(512), ar.f32(512)] for _ in range(2)]
                    PTH = ar.bf16(2 * 1024).rearrange("p (k s) -> p k s", k=2)
                    PLP = ar.bf16(2 * 2048).rearrange("p (k n) -> p k n", k=2)
                    PLG = [ARt[:, i * 4096:(i + 1) * 4096].bitcast(BF16).rearrange("p (k n) -> p k n", k=16) for i in range(2)]
                    tsl_h = slice(hf * 1024, (hf + 1) * 1024)
                    for kc in range(KC):
                        dma(X1H[:, kc, :], x1T[kc * 128:(kc + 1) * 128, tsl_h], [], [("X1H", kc)])
                    x1h_ids = [("X1H", kc) for kc in range(KC)]
                    prow = (l * epc + e_i) * 256
                    dma(PTH, kview(pT_in[prow:prow + 256, tsl_h], 2), [], ["PTH"], cast=True)
                    dma(PLP, kview(plp[l * 256:(l + 1) * 256, :], 2), [], ["PLP"], cast=True)
                    cnt = 0
                    for cg in range(4):
                        pl_ = cg % 2
                        dma(PLG[pl_], kview(plg[l * D:(l + 1) * D, cg * 512:(cg + 1) * 512], 16), [], [("PLG", pl_)], cast=True)
                        for tbh in range(8):
                            rows = slice(tbh * 128, (tbh + 1) * 128)
                            ba, bb = bank(), bank()
                            mm([(PS[ba][:], [(X1H[:, kc, rows], PLG[pl_][:, kc, :]) for kc in range(KC)])], x1h_ids + [("PLG", pl_)], ("ps", ba))
                            mm([(PS[bb][:], [(PTH[:, kc, rows], PLP[:, kc, cg * 512:(cg + 1) * 512]) for kc in range(2)])], ["PTH", "PLP"], ("ps", bb))
                            s_ = cnt % 2
                            cnt += 1
                            T1 = TT[s_][0]
                            act(T1, PS[ba][:], AF.Sigmoid, [("ps", ba)], [("T1", s_)])
                            tt(YACC[:, tbh, cg * 512:(cg + 1) * 512], T1, PS[bb][:], ALU.mult, [("T1", s_), ("ps", bb)], [("Y", tbh, cg)])
                    pg.barrier()
                    wcnt = 0
                    dcnt = 0
                    for ex in range(NE):
                        bs_ = ex % 2
                        bcast_load(BC[bs_], cwT_d[ex:ex + 1, tsl_h], ("BC", bs_))
                        r0 = (l * NE + ex) * D
                        for fp in range(4):
                            w_ = wcnt % 2
                            wcnt += 1
                            dma(WG[w_], kview(eg[r0:r0 + D, fp * 256:(fp + 1) * 256], 16), [], [("WG", w_)], cast=True)
                            dma(WU[w_], kview(eu[r0:r0 + D, fp * 256:(fp + 1) * 256], 16), [], [("WU", w_)], cast=True)
                            for fj in range(2):
                                fc = fp * 2 + fj
                                fsl = slice(fj * 128, (fj + 1) * 128)
                                for tg2 in range(2):
                                    tsl = slice(tg2 * 512, (tg2 + 1) * 512)
                                    bg_, bu_ = bank(), bank()
                                    mm([(PS[bg_][:], [(WG[w_][:, kc, fsl], X1H[:, kc, tsl]) for kc in range(KC)])], [("WG", w_)] + x1h_ids, ("ps", bg_))
                                    mm([(PS[bu_][:], [(WU[w_][:, kc, fsl], X1H[:, kc, tsl]) for kc in range(KC)])], [("WU", w_)] + x1h_ids, ("ps", bu_))
                                    s_ = cnt % 2
                                    cnt += 1
                                    T1, T2 = TT[s_]
                                    act(T1, PS[bg_][:], AF.Silu, [("ps", bg_)], [("T1", s_)])
                                    tt(T2, T1, PS[bu_][:], ALU.mult, [("T1", s_), ("ps", bu_)], [("T2", s_)])
                                    tt(HS[:, fc, tsl], T2, BC[bs_][:, tsl], ALU.mult, [("T2", s_), ("BC", bs_)], [("HS", fc, tg2)])
                        r1 = (l * NE + ex) * DE
                        for cg in range(4):
                            d_ = dcnt % 2
                            dcnt += 1
                            dma(WD[d_], kview(ed[r1:r1 + DE, cg * 512:(cg + 1) * 512], 8), [], [("WD", d_)], cast=True)
                            for tbh in range(8):
                                rows = slice(tbh * 128, (tbh + 1) * 128)
                                b = bank()
                                mm([(PS[b][:], [(HS[:, fc, rows], WD[d_][:, fc, :]) for fc in range(8)])], [("WD", d_)] + [("HS", fc, tbh // 4) for fc in range(8)], ("ps", b))
                                ysl = YACC[:, tbh, cg * 512:(cg + 1) * 512]
                                tt(ysl, ysl, PS[b][:], ALU.add, [("Y", tbh, cg), ("ps", b)], [("Y", tbh, cg)])
                    pg.barrier()
                    ar.reset()
                    G2 = ar.f32(2048)
                    B2 = ar.f32(2048)
                    X1R = [ar.f32(2048) for _ in range(2)]
                    X2B = [ar.bf16(2048) for _ in range(2)]
                    ST = ar.f32(24)
                    MV = ar.f32(2)
                    RS = ar.f32(1)
                    bcast_load(G2, l2g[l:l + 1, :], "G2")
                    bcast_load(B2, l2b[l:l + 1, :], "B2")
                    def ln2_load(tbh):
                        tb = hf * 8 + tbh
                        dma(X1R[tbh % 2], xb[tb * 128:(tb + 1) * 128, :], [], [("X1R", tbh % 2)])

                    def ln2_A(tbh):
                        tb = hf * 8 + tbh
                        rows = slice(tb * 128, (tb + 1) * 128)
                        s_ = tbh % 2
                        Y = YACC[:, tbh, :]
                        yid = ("Yr", tbh)
                        stt(Y, X1R[s_], ALPHA, Y, ALU.mult, ALU.add, [("X1R", s_)], [yid])
                        layer_norm(Y, yid, G2, B2, "G2", "B2", 2048, ST, MV, RS, "SM")
                        tt(Y, Y, B2, ALU.add, [yid, "B2"], [yid], eng="pool")
                        if last:
                            dma(out[e_i * S + tb * 128:e_i * S + (tb + 1) * 128, :], Y, [yid], [("out", tb)])
                        else:
                            dma(xa[rows, :], Y, [yid], [("xa", tb)])

                    def ln2_B(tbh):
                        tb = hf * 8 + tbh
                        rows = slice(tb * 128, (tb + 1) * 128)
                        s_ = tbh % 2
                        Y = YACC[:, tbh, :]
                        yid = ("Yr", tbh)
                        if not last:
                            for q4 in range(4):
                                b_ = bank()
                                tr([(PS[b_][:, j * 128:(j + 1) * 128], Y[:, (q4 * 4 + j) * 128:(q4 * 4 + j + 1) * 128]) for j in range(4)], [yid, "CST"], ("ps", b_))
                                cp(X2B[s_][:, q4 * 512:(q4 + 1) * 512], PS[b_][:], [("ps", b_)], [("X2B", s_, q4)], eng="act")
                            dma(xaT.rearrange("(k p) s -> p k s", p=128)[:, :, rows], X2B[s_].rearrange("p (k t) -> p k t", k=16), [("X2B", s_, q) for q in range(4)], [("xaT", tb)])

                    ln2_load(0)
                    for it in range(9):
                        if it + 1 < 8:
                            ln2_load(it + 1)
                        if it < 8:
                            ln2_A(it)
                        if it >= 1:
                            ln2_B(it - 1)
                    pg.barrier()

        pg.barrier()

        @block.tensor
        def _(e):
            pg.run("pe", e)

        @block.vector
        def _(e):
            pg.run("dve", e)

        @block.scalar
        def _(e):
            pg.run("act", e)

        @block.gpsimd
        def _(e):
            pg.run("pool", e)

        @block.sync
        def _(e):
            pg.run("sp", e)

    return nc


def make_consts():
    c = np.zeros((128, NCONST), np.float32)
    i = np.arange(128)
    c[:, 0:128] = np.eye(128, dtype=np.float32)
    c[:, 128:256] = (i[:, None] > i[None, :]).astype(np.float32)
    c[:, 256:384] = 1.0
    c[:, 384:512] = (i[:, None] <= i[None, :]).astype(np.float32)
    t = np.arange(512)
    for j in range(4):
        c[:, 512 + j * 512:512 + (j + 1) * 512] = ((t[None, :] - 128 * j) > i[:, None]).astype(np.float32)
    return c


def make_in_maps(inp, ncores, epc, depth=DEPTH):
    DEPTH = depth
    f = lambda a: np.ascontiguousarray(np.asarray(a, dtype=np.float32)[:depth]) if np.asarray(a).shape[0] == 4 and np.asarray(a).ndim >= 2 else np.ascontiguousarray(np.asarray(a, dtype=np.float32))
    x = f(inp["x"])
    p = f(inp["p"])
    shared = {
        "w_in": f(inp["w_in"]).reshape(DEPTH * D, INW),
        "w_out_a": f(inp["w_out_a"]).reshape(DEPTH * 1024, D),
        "w_out_b": f(inp["w_out_b"]).reshape(DEPTH * 1024, D),
        "w_o": f(inp["w_o"]).reshape(DEPTH * D, D),
        "exp_g": f(inp["exp_w_gate"]).reshape(DEPTH * NE * D, DE),
        "exp_u": f(inp["exp_w_up"]).reshape(DEPTH * NE * D, DE),
        "exp_d": f(inp["exp_w_down"]).reshape(DEPTH * NE * DE, D),
        "ple_g": f(inp["ple_w_gate"]).reshape(DEPTH * D, D),
        "ple_p": f(inp["ple_w_proj"]).reshape(DEPTH * 256, D),
        "router_w": f(inp["router_w"]),
        "router_bias": f(inp["router_bias"]).reshape(1, 16),
        "gmlp_ln_g": f(inp["gmlp_ln_g"]),
        "gmlp_ln_b": f(inp["gmlp_ln_b"]),
        "ln1_g": f(inp["ln1_g"]),
        "ln1_b": f(inp["ln1_b"]),
        "ln2_g": f(inp["ln2_g"]),
        "ln2_b": f(inp["ln2_b"]),
        "gmlp_bs": f(inp["gmlp_bs"]).reshape(DEPTH, 1024),
        "gmlp_wsT": np.ascontiguousarray(f(inp["gmlp_ws"]).transpose(0, 1, 3, 2)).reshape(DEPTH * 8 * 128, 128),
        "consts": make_consts(),
    }
    maps = []
    for c in range(ncores):
        bs = list(range(c * epc, (c + 1) * epc))
        m = dict(shared)
        m["x"] = np.ascontiguousarray(x[bs].reshape(epc * S, D))
        m["xT"] = np.ascontiguousarray(x[bs].transpose(0, 2, 1)).reshape(epc * D, S)
        m["pT"] = np.ascontiguousarray(p[:, bs].transpose(0, 1, 3, 2)).reshape(DEPTH * epc * 256, S)
        maps.append(m)
    return maps


def kernel(**inputs):
    nc = build(EPC, DEPTH)
    maps = make_in_maps(inputs, NCORES, EPC)
    res = run_bass_kernel_spmd(nc, maps, core_ids=list(range(NCORES)))
    outs = [np.asarray(r["out"]).reshape(EPC, S, D) for r in res.results]
    return np.concatenate(outs, axis=0).astype(np.float32)
```

```python
import numpy as np
import concourse.bass as bass
import concourse.mybir as mybir
from concourse.bass_utils import run_bass_kernel_spmd

F32 = mybir.dt.float32
BF16 = mybir.dt.bfloat16
AF = mybir.ActivationFunctionType
ALU = mybir.AluOpType
AX = mybir.AxisListType

S = 2048
D = 2048
KC = 16
INW = 9216
NE = 16
DE = 1024
DEPTH = 4
ALPHA = float((2 * DEPTH) ** 0.25)
EPS = 1e-5
SCALE = float(128 ** -0.5)
NCORES = 8
EPC = 1
NCONST = 128 * 4 + 4 * 512


class Prog:
    def __init__(self, esem, dsems):
        self.q = {n: [] for n in ("pe", "dve", "act", "pool", "sp")}
        self.seen = {n: {} for n in self.q}
        self.esem = esem
        self.ecount = {}
        self.epoch = 0
        self.dsems = dsems
        self.dcount = {}
        self.drr = {n: 0 for n in dsems}
        self.lastw = {}
        self.readers = {}

    def semh(self, k):
        if isinstance(k, tuple):
            return self.dsems[k[0]][k[1]]
        return self.esem[k]

    def op(self, eng, fn, reads=(), writes=(), dma=False):
        seen = self.seen[eng]
        waits = []

        def need(dep):
            if dep is None:
                return
            k, v = dep
            if eng == "pe" and isinstance(k, str) and k.startswith("pe#"):
                return
            if seen.get(k, 0) >= v:
                return
            seen[k] = v
            waits.append((k, v))

        for r in reads:
            need(self.lastw.get(r))
        for w in writes:
            need(self.lastw.get(w))
            for k, v in self.readers.get(w, {}).items():
                need((k, v))
        if dma:
            lst = self.dsems[eng]
            i = self.drr[eng]
            self.drr[eng] = (i + 1) % len(lst)
            k = (eng, i)
            prev = self.dcount.get(k, 0)
            if prev:
                need((k, prev))
            self.dcount[k] = prev + 16
            comp = (k, prev + 16)
        else:
            ek = eng + "#" + str(self.epoch)
            self.ecount[ek] = self.ecount.get(ek, 0) + 1
            comp = (ek, self.ecount[ek])
        for r in reads:
            d = self.readers.setdefault(r, {})
            d[comp[0]] = max(d.get(comp[0], 0), comp[1])
        for w in writes:
            self.lastw[w] = comp
            self.readers[w] = {}
        self.q[eng].append((waits, fn, comp))

    def barrier(self):
        allk = [(k, v) for k, v in self.ecount.items() if v > 0]
        allk += [(k, v) for k, v in self.dcount.items() if v > 0]
        for eng in self.q:
            seen = self.seen[eng]
            waits = []
            for k, v in allk:
                if isinstance(k, str) and k.split("#")[0] == eng:
                    continue
                if seen.get(k, 0) >= v:
                    continue
                seen[k] = v
                waits.append((k, v))
            if waits:
                self.q[eng].append((waits, None, None))
        self.lastw.clear()
        self.readers.clear()

    def run(self, eng, e):
        for waits, fn, comp in self.q[eng]:
            for k, v in waits:
                e.wait_ge(self.semh(k), v)
            if fn is None:
                continue
            ins = fn(e)
            k, v = comp
            ins.then_inc(self.semh(k), 16 if isinstance(k, tuple) else 1)


def build(epc, depth_run, wd=DEPTH):
    nc = bass.Bass("TRN2", target_bir_lowering=False)
    dt = nc.dram_tensor

    def ein(name, shape):
        return dt(name, shape, F32, kind="ExternalInput").ap()

    x_in = ein("x", [epc * S, D])
    xT_in = ein("xT", [epc * D, S])
    pT_in = ein("pT", [wd * epc * 256, S])
    w_in = ein("w_in", [wd * D, INW])
    w_oa = ein("w_out_a", [wd * 1024, D])
    w_ob = ein("w_out_b", [wd * 1024, D])
    w_o = ein("w_o", [wd * D, D])
    eg = ein("exp_g", [wd * NE * D, DE])
    eu = ein("exp_u", [wd * NE * D, DE])
    ed = ein("exp_d", [wd * NE * DE, D])
    plg = ein("ple_g", [wd * D, D])
    plp = ein("ple_p", [wd * 256, D])
    rw = ein("router_w", [D, 16])
    rb = ein("router_bias", [1, 16])
    gg = ein("gmlp_ln_g", [wd, 1024])
    gb = ein("gmlp_ln_b", [wd, 1024])
    l1g = ein("ln1_g", [wd, D])
    l1b = ein("ln1_b", [wd, D])
    l2g = ein("ln2_g", [wd, D])
    l2b = ein("ln2_b", [wd, D])
    bsd = ein("gmlp_bs", [wd, 1024])
    wsT = ein("gmlp_wsT", [wd * 8 * 128, 128])
    cst = ein("consts", [128, NCONST])
    out = dt("out", [epc * S, D], F32, kind="ExternalOutput").ap()
    xa = dt("xa", [S, D], F32).ap()
    xb = dt("xb", [S, D], F32).ap()
    xaT = dt("xaT", [D, S], BF16).ap()
    x1T = dt("x1T", [D, S], BF16).ap()
    mixT = dt("mixT", [D, S], BF16).ap()
    cwT_d = dt("cwT_d", [16, S], F32).ap()

    import contextlib
    with contextlib.ExitStack() as st:
        R1t = st.enter_context(nc.sbuf_tensor("R1", [128, 32768], BF16))
        R2t = st.enter_context(nc.sbuf_tensor("R2", [128, 32768], BF16))
        ARt = st.enter_context(nc.sbuf_tensor("AR", [128, 15360], F32))
        CSTt = st.enter_context(nc.sbuf_tensor("CST", [128, NCONST], F32))
        CSTBt = st.enter_context(nc.sbuf_tensor("CSTB", [128, 256], BF16))
        PS = [st.enter_context(nc.psum_tensor(f"ps{i}", [128, 512], F32)) for i in range(8)]
        esem = {f"{n}#{ep}": st.enter_context(nc.semaphore(f"s_{n}_{ep}")) for n in ("pe", "dve", "act", "pool") for ep in range(epc)}
        dsems = {
            "sp": [st.enter_context(nc.semaphore(f"d_sp{i}")) for i in range(8)],
            "pool": [st.enter_context(nc.semaphore(f"d_pl{i}")) for i in range(8)],
        }
        block = st.enter_context(nc.Block())
        pg = Prog(esem, dsems)

        R1f = R1t[:]
        R2f = R2t[:]
        XT = R1f.rearrange("p (k s) -> p k s", k=16)
        R2v = R2f.rearrange("p (k s) -> p k s", k=16)
        IDENT = CSTt[:, 0:128]
        TRI = CSTt[:, 128:256]
        ONES = CSTt[:, 256:384]
        TRIU = CSTt[:, 384:512]
        MASKD = [CSTt[:, 512 + j * 512:512 + (j + 1) * 512] for j in range(4)]

        class Arena:
            def __init__(self):
                self.off = 0

            def reset(self):
                self.off = 0

            def f32(self, n):
                a = ARt[:, self.off:self.off + n]
                self.off += n
                assert self.off <= 15360, self.off
                return a

            def bf16(self, n):
                assert n % 2 == 0
                a = ARt[:, self.off:self.off + n // 2].bitcast(BF16)
                self.off += n // 2
                assert self.off <= 15360, self.off
                return a

        ar = Arena()
        uid = [0]

        def nid(tag):
            uid[0] += 1
            return (tag, uid[0])

        def mm(groups, reads, wid):
            def fn(e, groups=groups):
                ins = None
                for o, pairs in groups:
                    n = len(pairs)
                    for i, (l, r) in enumerate(pairs):
                        ins = e.matmul(o, l, r, start=(i == 0), stop=(i == n - 1))
                return ins
            pg.op("pe", fn, reads=reads, writes=[wid])

        def tr(groups, reads, wid):
            def fn(e, groups=groups):
                ins = None
                for o, i_ in groups:
                    ins = e.transpose(o, i_, IDENT)
                return ins
            pg.op("pe", fn, reads=reads, writes=[wid])

        def act(o, i_, func, reads, writes, scale=1.0, bias=0.0):
            def fn(e, o=o, i_=i_):
                return e.activation(out=o, in_=i_, func=func, bias=bias, scale=scale)
            pg.op("act", fn, reads=reads, writes=writes)

        def tt(o, a, b, op, reads, writes, eng="dve"):
            def fn(e, o=o, a=a, b=b):
                return e.tensor_tensor(out=o, in0=a, in1=b, op=op)
            pg.op(eng, fn, reads=reads, writes=writes)

        def ts(o, a, s1, s2, op0, op1, reads, writes):
            def fn(e, o=o, a=a):
                if s2 is None:
                    return e.tensor_scalar(out=o, in0=a, scalar1=s1, scalar2=None, op0=op0)
                return e.tensor_scalar(out=o, in0=a, scalar1=s1, scalar2=s2, op0=op0, op1=op1)
            pg.op("dve", fn, reads=reads, writes=writes)

        def stt(o, a, sc, b, op0, op1, reads, writes):
            def fn(e, o=o, a=a, b=b):
                return e.scalar_tensor_tensor(out=o, in0=a, scalar=sc, in1=b, op0=op0, op1=op1)
            pg.op("dve", fn, reads=reads, writes=writes)

        def cp(o, i_, reads, writes, eng="dve"):
            if eng == "act":
                def fn(e, o=o, i_=i_):
                    return e.copy(out=o, in_=i_)
            else:
                def fn(e, o=o, i_=i_):
                    return e.tensor_copy(out=o, in_=i_)
            pg.op(eng, fn, reads=reads, writes=writes)

        def red(o, i_, op, reads, writes):
            def fn(e, o=o, i_=i_):
                return e.tensor_reduce(out=o, in_=i_, axis=AX.X, op=op)
            pg.op("dve", fn, reads=reads, writes=writes)

        def dma(o, i_, reads, writes, cast=False):
            eng = "pool" if cast else "sp"

            def fn(e, o=o, i_=i_):
                return e.dma_start(out=o, in_=i_)
            pg.op(eng, fn, reads=reads, writes=writes, dma=True)

        def bcast_load(dst, src_row, wid):
            dma(dst, src_row.partition_broadcast(128)[:, 0, :], [], [wid])

        def kview(ap2d, kc):
            return ap2d.rearrange("(k p) n -> p k n", p=128)

        psr = [0]

        def bank(lo=0, hi=8):
            b = lo + psr[0] % (hi - lo)
            psr[0] += 1
            return b

        def gelu_a(ps_ap, psid, T1, t1id):
            act(T1, ps_ap, AF.Square, [psid], [t1id])
            ts(T1, T1, 0.044715, 1.0, ALU.mult, ALU.add, [t1id], [t1id])
            tt(T1, T1, ps_ap, ALU.mult, [t1id, psid], [t1id])

        def gelu_b(ps_ap, psid, dst, dstid, T1, T2, t1id, t2id):
            act(T2, T1, AF.Sigmoid, [t1id], [t2id], scale=1.5957691216057308)
            tt(dst, T2, ps_ap, ALU.mult, [t2id, psid], [dstid])

        def gelu(ps_ap, psid, dst, dstid, T1, T2, t1id, t2id):
            gelu_a(ps_ap, psid, T1, t1id)
            gelu_b(ps_ap, psid, dst, dstid, T1, T2, t1id, t2id)

        def layer_norm(T, tid, G, B, gid, bid, n, ST, MV, RS, smid):
            nch = n // 512
            for c in range(nch):
                def fn(e, c=c):
                    return e.bn_stats(out=ST[:, c * 6:(c + 1) * 6], in_=T[:, c * 512:(c + 1) * 512])
                pg.op("dve", fn, reads=[tid], writes=[smid])
            def fn2(e):
                return e.bn_aggr(out=MV, in_=ST[:, 0:nch * 6])
            pg.op("dve", fn2, reads=[smid], writes=[smid])
            act(RS, MV[:, 1:2], AF.Sqrt, [smid], ["RSQ"], bias=EPS)
            def fn3(e):
                return e.reciprocal(out=RS, in_=RS)
            pg.op("dve", fn3, reads=["RSQ"], writes=[smid])
            ts(MV[:, 1:2], MV[:, 0:1], RS, -1.0, ALU.mult, ALU.mult, [smid], [smid])
            def fn4(e):
                return e.activation(out=T, in_=T, func=AF.Identity, bias=MV[:, 1:2], scale=RS)
            pg.op("act", fn4, reads=[tid, smid], writes=[tid])
            tt(T, T, G, ALU.mult, [tid, gid], [tid], eng="pool")

        dma(CSTt[:], cst[:, :], [], ["CST"])
        cp(CSTBt[:, 0:256], CSTt[:, 128:384], ["CST"], ["CSTB"])
        TRIB = CSTBt[:, 0:128]
        ONESB = CSTBt[:, 128:256]
        pg.barrier()

        for e_i in range(epc):
            pg.epoch = e_i
            for l in range(depth_run):
                last = (l == depth_run - 1)
                wl = w_in[l * D:(l + 1) * D, :]
                for kc in range(KC):
                    if l == 0:
                        dma(XT[:, kc, :], xT_in[e_i * D + kc * 128:e_i * D + (kc + 1) * 128, :], [], [("XT", kc)], cast=True)
                    else:
                        dma(XT[:, kc, :], xaT[kc * 128:(kc + 1) * 128, :], [], [("XT", kc)])
                xt_ids = [("XT", kc) for kc in range(KC)]
                ar.reset()
                WS = [ar.bf16(16 * 512).rearrange("p (k n) -> p k n", k=16) for _ in range(2)]
                TT = [[ar.f32(512), ar.f32(512)] for _ in range(2)]
                for grp in range(2):
                    dma(WS[grp], kview(wl[:, grp * 512:(grp + 1) * 512], 16), [], [("WS", grp)], cast=True)
                pend = None
                cnt = 0
                for grp in range(2):
                    wsid = ("WS", grp)
                    for cc in range(4):
                        h = grp * 4 + cc
                        for tg in range(4):
                            b = bank()
                            mm([(PS[b][:], [(WS[grp][:, kc, cc * 128:(cc + 1) * 128], XT[:, kc, tg * 512:(tg + 1) * 512]) for kc in range(KC)])],
                               [wsid] + xt_ids, ("ps", b))
                            s_ = cnt % 2
                            cnt += 1
                            gelu_a(PS[b][:], ("ps", b), TT[s_][0], ("T1", s_))
                            if pend is not None:
                                gelu_b(*pend)
                            pend = (PS[b][:], ("ps", b), R2v[:, h, tg * 512:(tg + 1) * 512], ("R2", h, tg),
                                    TT[s_][0], TT[s_][1], ("T1", s_), ("T2", s_))
                gelu_b(*pend)
                pg.barrier()
                ar.reset()
                VW = R2f[:, 16384:32768].rearrange("p (k n) -> p k n", k=16)
                for hf in range(2):
                    dma(VW[:, :, hf * 512:(hf + 1) * 512], kview(wl[:, 1024 + hf * 512:1024 + (hf + 1) * 512], 16), [], [("VW", hf)], cast=True)
                GG = ar.f32(1024)
                GB_ = ar.f32(1024)
                BSB = ar.f32(1024)
                WSF = ar.f32(1024)
                WSB = ar.bf16(1024)
                VG = [ar.f32(1024) for _ in range(2)]
                VC = [ar.bf16(1024) for _ in range(2)]
                T1s = [[ar.f32(512), ar.f32(512)] for _ in range(2)]
                FB = [ar.f32(512) for _ in range(2)]
                ST = ar.f32(24)
                MV = ar.f32(2)
                RS = ar.f32(1)
                bcast_load(GG, gg[l:l + 1, :], "GG")
                bcast_load(GB_, gb[l:l + 1, :], "GB")
                bcast_load(BSB, bsd[l:l + 1, :], "BSB")
                dma(WSF.rearrange("p (h t) -> p h t", h=8), wsT[l * 1024:(l + 1) * 1024, :].rearrange("(h p) t -> p h t", p=128), [], ["WSF"])
                for h in range(8):
                    tt(WSB[:, h * 128:(h + 1) * 128], WSF[:, h * 128:(h + 1) * 128], TRIU, ALU.mult, ["WSF", "CST"], [("WSB", h)])
                wsb_ids = [("WSB", h) for h in range(8)]
                cnt = [0]

                def s1b_A(c):
                    s_ = c % 2
                    vgid = ("VG", s_)
                    for hf in range(2):
                        b = bank()
                        mm([(PS[b][:], [(XT[:, kc, c * 128:(c + 1) * 128], VW[:, kc, hf * 512:(hf + 1) * 512]) for kc in range(KC)])],
                           [("VW", hf)] + xt_ids, ("ps", b))
                        t_ = cnt[0] % 2
                        cnt[0] += 1
                        gelu(PS[b][:], ("ps", b), VG[s_][:, hf * 512:(hf + 1) * 512], vgid,
                             T1s[t_][0], T1s[t_][1], ("T1", t_), ("T2", t_))
                    layer_norm(VG[s_], vgid, GG, GB_, "GG", "GB", 1024, ST, MV, RS, "SM")
                    tt(VC[s_], VG[s_], GB_, ALU.add, [vgid, "GB"], [("VC", s_)], eng="pool")

                def s1b_B(c):
                    s_ = c % 2
                    for hh in range(2):
                        b = bank()
                        mm([(PS[b][:, j * 128:(j + 1) * 128], [(VC[s_][:, (hh * 4 + j) * 128:(hh * 4 + j + 1) * 128], WSB[:, (hh * 4 + j) * 128:(hh * 4 + j + 1) * 128])]) for j in range(4)],
                           [("VC", s_)] + wsb_ids, ("ps", b))
                        f_ = hh
                        F1 = FB[f_]
                        tt(F1, PS[b][:], BSB[:, hh * 512:(hh + 1) * 512], ALU.add, [("ps", b), "BSB"], [("F1", f_)])
                        dst = R2v[:, hh * 4:(hh + 1) * 4, c * 128:(c + 1) * 128]
                        ids = [("R2", hh * 4 + j, c // 4) for j in range(4)]
                        def fn(e, dst=dst, F1=F1):
                            return e.tensor_tensor(out=dst, in0=F1.rearrange("p (h t) -> p h t", h=4), in1=dst, op=ALU.mult)
                        pg.op("dve", fn, reads=[("F1", f_)] + ids, writes=ids)

                for it in range(17):
                    if it < 16:
                        s1b_A(it)
                    if it >= 1:
                        s1b_B(it - 1)
                pg.barrier()
                ar.reset()
                WQ = ar.bf16(16 * 128).rearrange("p (k n) -> p k n", k=16)
                WK = ar.bf16(16 * 128).rearrange("p (k n) -> p k n", k=16)
                WVh = ar.bf16(16 * 128).rearrange("p (k n) -> p k n", k=16)
                QT = ar.bf16(2048)
                KT = ar.bf16(2048)
                VH = ar.bf16(2048)
                E_ = [ar.f32(512) for _ in range(2)]
                SP = [ar.bf16(512) for _ in range(3)]
                LA = [ar.f32(512) for _ in range(2)]
                A_ = [ar.bf16(512) for _ in range(3)]
                for h in range(8):
                    dma(WQ, kview(wl[:, 2048 + h * 128:2048 + (h + 1) * 128], 16), [], ["WQ"], cast=True)
                    dma(WK, kview(wl[:, 3072 + h * 128:3072 + (h + 1) * 128], 16), [], ["WK"], cast=True)
                    dma(WVh, kview(wl[:, 4096 + h * 128:4096 + (h + 1) * 128], 16), [], ["WV"], cast=True)
                    for tg in range(4):
                        b = bank(0, 8)
                        mm([(PS[b][:], [(WQ[:, kc, :], XT[:, kc, tg * 512:(tg + 1) * 512]) for kc in range(KC)])], ["WQ"] + xt_ids, ("ps", b))
                        cp(QT[:, tg * 512:(tg + 1) * 512], PS[b][:], [("ps", b)], [("QT", tg)], eng="act")
                        b = bank(0, 8)
                        mm([(PS[b][:], [(WK[:, kc, :], XT[:, kc, tg * 512:(tg + 1) * 512]) for kc in range(KC)])], ["WK"] + xt_ids, ("ps", b))
                        cp(KT[:, tg * 512:(tg + 1) * 512], PS[b][:], [("ps", b)], [("KT", tg)])
                        b = bank(0, 8)
                        mm([(PS[b][:, j * 128:(j + 1) * 128], [(XT[:, kc, (tg * 4 + j) * 128:(tg * 4 + j + 1) * 128], WVh[:, kc, :]) for kc in range(KC)]) for j in range(4)],
                           ["WV"] + xt_ids, ("ps", b))
                        cp(VH[:, tg * 512:(tg + 1) * 512], PS[b][:], [("ps", b)], [("VH", tg)], eng="act")
                    for g in range(4):
                        t0 = g * 512
                        ob = 6 + g % 2
                        tiles = list(range(4 * g + 3, -1, -1))
                        n = len(tiles)

                        def stZ(i, g=g, t0=t0, tiles=tiles):
                            kc = tiles[i]
                            zb = 1 + i % 3
                            mm([(PS[zb][:], [(KT[:, kc * 128:(kc + 1) * 128], QT[:, t0:t0 + 512])])], [("KT", kc // 4), ("QT", g)], ("ps", zb))

                        def stA(i, g=g, tiles=tiles):
                            kc = tiles[i]
                            zb = 1 + i % 3
                            e_, sp_ = i % 2, i % 3
                            diag = kc >= 4 * g
                            j = kc - 4 * g
                            act(E_[e_], PS[zb][:], AF.Exp, [("ps", zb)], [("E", e_)], scale=SCALE)
                            act(SP[sp_], E_[e_], AF.Ln, [("E", e_)], [("SP", sp_)], bias=1.0)
                            if diag:
                                tt(SP[sp_], SP[sp_], MASKD[j], ALU.mult, [("SP", sp_), "CST"], [("SP", sp_)])

                        def stB(i, g=g, tiles=tiles):
                            kc = tiles[i]
                            zb = 1 + i % 3
                            sb = 4 + i % 2
                            sp_, la_, a_ = i % 3, i % 2, i % 3
                            diag = kc >= 4 * g
                            j = kc - 4 * g
                            rb = 0
                            if i > 0:
                                def fnr_(e, rb=rb, i=i):
                                    return e.matmul(PS[rb][:], ONESB, SP[(i - 1) % 3], start=(i == 1), stop=True, skip_group_check=True)
                                pg.op("pe", fnr_, reads=[("SP", (i - 1) % 3), "CSTB"], writes=[("ps", rb)])
                            mm([(PS[sb][:], [(TRIB, SP[sp_])])], [("SP", sp_), "CSTB"], ("ps", sb))
                            stt(LA[la_], PS[zb][:], SCALE, SP[sp_], ALU.mult, ALU.subtract, [("ps", zb), ("SP", sp_)], [("LA", la_)])
                            tt(LA[la_], LA[la_], PS[sb][:], ALU.subtract, [("LA", la_), ("ps", sb)], [("LA", la_)])
                            if i > 0:
                                tt(LA[la_], LA[la_], PS[rb][:], ALU.subtract, [("LA", la_), ("ps", rb)], [("LA", la_)])
                            act(A_[a_], LA[la_], AF.Exp, [("LA", la_)], [("A", a_)])
                            if diag:
                                tt(A_[a_], A_[a_], MASKD[j], ALU.mult, [("A", a_), "CST"], [("A", a_)])

                        def stC(i, ob=ob, tiles=tiles, n=n):
                            kc = tiles[i]
                            a_ = i % 3
                            def fn(e, ob=ob, kc=kc, a_=a_, i=i, n=n):
                                return e.matmul(PS[ob][:], VH[:, kc * 128:(kc + 1) * 128], A_[a_], start=(i == 0), stop=(i == n - 1), skip_group_check=True)
                            pg.op("pe", fn, reads=[("VH", kc // 4), ("A", a_)], writes=[("ps", ob)])

                        stZ(0)
                        for it in range(n + 2):
                            if it < n:
                                stA(it)
                            if 0 <= it - 1 < n:
                                stB(it - 1)
                            if 0 <= it - 2 < n:
                                stC(it - 2)
                            if it + 1 < n:
                                stZ(it + 1)
                        cp(R2v[:, 8 + h, t0:t0 + 512], PS[ob][:], [("ps", ob)], [("R2", 8 + h, g)], eng="act")
                pg.barrier()
                ar.reset()
                WGA = [ar.bf16(16 * 256).rearrange("p (k n) -> p k n", k=16) for _ in range(2)]
                WGB = [ar.bf16(16 * 256).rearrange("p (k n) -> p k n", k=16) for _ in range(2)]
                WA = [ar.bf16(8 * 256).rearrange("p (k n) -> p k n", k=8) for _ in range(2)]
                WB = [ar.bf16(8 * 256).rearrange("p (k n) -> p k n", k=8) for _ in range(2)]
                TT = [[ar.f32(512), ar.f32(512)] for _ in range(2)]
                MST = [ar.bf16(2048) for _ in range(1)]
                cnt = 0
                for cgp in range(8):
                    w_ = cgp % 2
                    c0 = cgp * 256
                    dma(WGA[w_], kview(wl[:, 5120 + c0:5120 + c0 + 256], 16), [], [("WGA", w_)], cast=True)
                    dma(WA[w_], kview(w_oa[l * 1024:(l + 1) * 1024, c0:c0 + 256], 8), [], [("WA", w_)], cast=True)
                    dma(WGB[w_], kview(wl[:, 7168 + c0:7168 + c0 + 256], 16), [], [("WGB", w_)], cast=True)
                    dma(WB[w_], kview(w_ob[l * 1024:(l + 1) * 1024, c0:c0 + 256], 8), [], [("WB", w_)], cast=True)
                    for cc in range(2):
                        c = cgp * 2 + cc
                        m_ = 0
                        for tg in range(4):
                            tsl = slice(tg * 512, (tg + 1) * 512)
                            csl = slice(cc * 128, (cc + 1) * 128)
                            bga, bya, bgb, byb = bank(), bank(), bank(), bank()
                            mm([(PS[bga][:], [(WGA[w_][:, kc, csl], XT[:, kc, tsl]) for kc in range(16)])], [("WGA", w_)] + xt_ids, ("ps", bga))
                            mm([(PS[bya][:], [(WA[w_][:, kc, csl], R2v[:, kc, tsl]) for kc in range(8)])], [("WA", w_)] + [("R2", kc, tg) for kc in range(8)], ("ps", bya))
                            mm([(PS[bgb][:], [(WGB[w_][:, kc, csl], XT[:, kc, tsl]) for kc in range(16)])], [("WGB", w_)] + xt_ids, ("ps", bgb))
                            mm([(PS[byb][:], [(WB[w_][:, kc, csl], R2v[:, 8 + kc, tsl]) for kc in range(8)])], [("WB", w_)] + [("R2", 8 + kc, tg) for kc in range(8)], ("ps", byb))
                            s_ = cnt % 2
                            cnt += 1
                            T1, T2 = TT[s_]
                            act(T1, PS[bga][:], AF.Sigmoid, [("ps", bga)], [("T1", s_)])
                            act(T2, PS[bgb][:], AF.Sigmoid, [("ps", bgb)], [("T2", s_)])
                            tt(T1, T1, PS[bya][:], ALU.mult, [("T1", s_), ("ps", bya)], [("T1", s_)])
                            tt(T2, T2, PS[byb][:], ALU.mult, [("T2", s_), ("ps", byb)], [("T2", s_)])
                            tt(MST[m_][:, tsl], T1, T2, ALU.add, [("T1", s_), ("T2", s_)], [("MST", m_, tg)])
                        dma(mixT[c * 128:(c + 1) * 128, :], MST[m_], [("MST", m_, tg) for tg in range(4)], [("mixT", c)])
                pg.barrier()
                ar.reset()
                WO = XT
                for kc in range(KC):
                    dma(WO[:, kc, :], w_o[l * D + kc * 128:l * D + (kc + 1) * 128, :], [], [("WO", kc)], cast=True)
                    dma(R2v[:, kc, :], mixT[kc * 128:(kc + 1) * 128, :], [], [("MX", kc)])
                wo_ids = [("WO", kc) for kc in range(KC)]
                mx_ids = [("MX", kc) for kc in range(KC)]
                Tb = [ar.f32(2048) for _ in range(3)]
                G1 = ar.f32(2048)
                B1 = ar.f32(2048)
                X1F = ar.f32(2048)
                X1B = ar.bf16(2048)
                RW = ar.f32(256)
                RB = ar.f32(16)
                CWT = [ar.f32(128) for _ in range(2)]
                ST = ar.f32(24)
                MV = ar.f32(2)
                RS = ar.f32(1)
                sm = [ar.f32(16) for _ in range(8)]
                AFF, SEL, EQ, SEL2, EM, W_, CW, _u = sm
                CWs = [CW, _u]
                M1 = ar.f32(4)
                M2 = ar.f32(4)
                GS = ar.f32(4)
                GMK = ar.f32(4)
                GM = ar.f32(1)
                WSUM = ar.f32(1)
                RWS = ar.f32(1)
                bcast_load(G1, l1g[l:l + 1, :], "G1")
                bcast_load(B1, l1b[l:l + 1, :], "B1")
                bcast_load(RB, rb[0:1, :], "RB")
                dma(RW.rearrange("p (k e) -> p k e", k=16), rw.rearrange("(k p) e -> p k e", p=128), [], ["RW"])

                def s4_load(tb):
                    T = Tb[tb % 3]
                    if l == 0:
                        dma(T, x_in[e_i * S + tb * 128:e_i * S + (tb + 1) * 128, :], [], [("T", tb % 3)])
                    else:
                        dma(T, xa[tb * 128:(tb + 1) * 128, :], [], [("T", tb % 3)])

                def s4_A(tb):
                    T = Tb[tb % 3]
                    tid = ("T", tb % 3)
                    rows = slice(tb * 128, (tb + 1) * 128)
                    for cg in range(4):
                        mm([(PS[cg][:], [(R2v[:, kc, rows], WO[:, kc, cg * 512:(cg + 1) * 512]) for kc in range(KC)])], mx_ids + wo_ids, ("ps", cg))
                        stt(T[:, cg * 512:(cg + 1) * 512], T[:, cg * 512:(cg + 1) * 512], ALPHA, PS[cg][:], ALU.mult, ALU.add, [tid, ("ps", cg)], [tid])
                    layer_norm(T, tid, G1, B1, "G1", "B1", 2048, ST, MV, RS, "SM")
                    tt(T, T, B1, ALU.add, [tid, "B1"], [tid], eng="pool")
                    dma(xb[rows, :], T, [tid], [("xb", tb)])

                def s4_B(tb):
                    T = Tb[tb % 3]
                    tid = ("T", tb % 3)
                    rows = slice(tb * 128, (tb + 1) * 128)
                    for q4 in range(4):
                        b_ = 4 + q4 % 2
                        tr([(PS[b_][:, j * 128:(j + 1) * 128], T[:, (q4 * 4 + j) * 128:(q4 * 4 + j + 1) * 128]) for j in range(4)], [tid, "CST"], ("ps", b_))
                        cp(X1F[:, q4 * 512:(q4 + 1) * 512], PS[b_][:], [("ps", b_)], [("X1F", q4)], eng="act")
                        cp(X1B[:, q4 * 512:(q4 + 1) * 512], X1F[:, q4 * 512:(q4 + 1) * 512], [("X1F", q4)], [("X1B", q4)])
                    dma(x1T.rearrange("(k p) s -> p k s", p=128)[:, :, rows], X1B.rearrange("p (k t) -> p k t", k=16), [("X1B", q) for q in range(4)], [("x1T", tb)])
                    X1Fv = X1F.rearrange("p (k t) -> p k t", k=16)
                    RWv = RW.rearrange("p (k e) -> p k e", k=16)
                    mm([(PS[6][:, 0:16], [(X1Fv[:, kc, :], RWv[:, kc, :]) for kc in range(KC)])], [("X1F", q) for q in range(4)] + ["RW"], ("ps", 6))
                    act(AFF, PS[6][:, 0:16], AF.Sigmoid, [("ps", 6)], ["AFF"])
                    tt(SEL, AFF, RB, ALU.add, ["AFF", "RB"], ["SEL"])
                    red(M1, SEL.rearrange("p (g e) -> p g e", g=4), ALU.max, ["SEL"], ["M1"])
                    for g_ in range(4):
                        ts(EQ[:, g_ * 4:(g_ + 1) * 4], SEL[:, g_ * 4:(g_ + 1) * 4], M1[:, g_:g_ + 1], None, ALU.is_equal, None, ["SEL", "M1"], ["EQ"])
                    stt(SEL2, EQ, -1.0e9, SEL, ALU.mult, ALU.add, ["EQ", "SEL"], ["SEL2"])
                    red(M2, SEL2.rearrange("p (g e) -> p g e", g=4), ALU.max, ["SEL2"], ["M2"])
                    tt(GS, M1, M2, ALU.add, ["M1", "M2"], ["GS"])
                    red(GM, GS, ALU.max, ["GS"], ["GM"])
                    ts(GMK, GS, GM[:, 0:1], None, ALU.is_equal, None, ["GS", "GM"], ["GMK"])
                    for g_ in range(4):
                        ts(EM[:, g_ * 4:(g_ + 1) * 4], SEL[:, g_ * 4:(g_ + 1) * 4], M2[:, g_:g_ + 1], GMK[:, g_:g_ + 1], ALU.is_ge, ALU.mult, ["SEL", "M2", "GMK"], ["EM"])
                    tt(W_, AFF, EM, ALU.mult, ["AFF", "EM"], ["W"])
                    red(WSUM, W_, ALU.add, ["W"], ["WSUM"])
                    def fnr(e):
                        return e.reciprocal(out=RWS, in_=WSUM)
                    pg.op("dve", fnr, reads=["WSUM"], writes=["RWS"])
                    ts(CWs[tb % 2], W_, RWS[:, 0:1], None, ALU.mult, None, ["W", "RWS"], [("CW", tb % 2)])

                def s4_C(tb):
                    rows = slice(tb * 128, (tb + 1) * 128)
                    c_ = tb % 2
                    tr([(PS[7][0:16, 0:128], CWs[c_])], [("CW", c_), "CST"], ("ps", 7))
                    cp(CWT[c_][0:16, :], PS[7][0:16, 0:128], [("ps", 7)], [("CWT", c_)], eng="act")
                    dma(cwT_d[:, rows], CWT[c_][0:16, :], [("CWT", c_)], [("cwT_d", tb)])

                s4_load(0)
                for it in range(18):
                    if it + 1 < 16:
                        s4_load(it + 1)
                    if it < 16:
                        s4_A(it)
                    if 1 <= it <= 16:
                        s4_B(it - 1)
                    if 2 <= it <= 17:
                        s4_C(it - 2)
                pg.barrier()
                YACC = R1f.bitcast(F32).rearrange("p (t n) -> p t n", t=8)
                X1H = R2f[:, 0:16384].rearrange("p (k s) -> p k s", k=16)
                HS = R2f[:, 16384:24576].rearrange("p (k s) -> p k s", k=8)
                WD = [R2f[:, 24576 + i * 4096:24576 + (i + 1) * 4096].rearrange("p (k n) -> p k n", k=8) for i in range(2)]
                for hf in range(2):
                    ar.reset()
                    WG = [ar.bf16(16 * 256).rearrange("p (k n) -> p k n", k=16) for _ in range(2)]
                    WU = [ar.bf16(16 * 256).rearrange("p (k n) -> p k n", k=16) for _ in range(2)]
                    BC = [ar.f32(1024) for _ in range(2)]
                    TT = [[ar.f32(512), ar.f32(512)] for _ in range(2)]
                    PTH = ar.bf16(2 * 1024).rearrange("p (k s) -> p k s", k=2)
                    PLP = ar.bf16(2 * 2048).rearrange("p (k n) -> p k n", k=2)
                    PLG = [ARt[:, i * 4096:(i + 1) * 4096].bitcast(BF16).rearrange("p (k n) -> p k n", k=16) for i in range(2)]
                    tsl_h = slice(hf * 1024, (hf + 1) * 1024)
                    for kc in range(KC):
                        dma(X1H[:, kc, :], x1T[kc * 128:(kc + 1) * 128, tsl_h], [], [("X1H", kc)])
                    x1h_ids = [("X1H", kc) for kc in range(KC)]
                    prow = (l * epc + e_i) * 256
                    dma(PTH, kview(pT_in[prow:prow + 256, tsl_h], 2), [], ["PTH"], cast=True)
                    dma(PLP, kview(plp[l * 256:(l + 1) * 256, :], 2), [], ["PLP"], cast=True)
                    cnt = 0
                    for cg in range(4):
                        pl_ = cg % 2
                        dma(PLG[pl_], kview(plg[l * D:(l + 1) * D, cg * 512:(cg + 1) * 512], 16), [], [("PLG", pl_)], cast=True)
                        for tbh in range(8):
                            rows = slice(tbh * 128, (tbh + 1) * 128)
                            ba, bb = bank(), bank()
                            mm([(PS[ba][:], [(X1H[:, kc, rows], PLG[pl_][:, kc, :]) for kc in range(KC)])], x1h_ids + [("PLG", pl_)], ("ps", ba))
                            mm([(PS[bb][:], [(PTH[:, kc, rows], PLP[:, kc, cg * 512:(cg + 1) * 512]) for kc in range(2)])], ["PTH", "PLP"], ("ps", bb))
                            s_ = cnt % 2
                            cnt += 1
                            T1 = TT[s_][0]
                            act(T1, PS[ba][:], AF.Sigmoid, [("ps", ba)], [("T1", s_)])
                            tt(YACC[:, tbh, cg * 512:(cg + 1) * 512], T1, PS[bb][:], ALU.mult, [("T1", s_), ("ps", bb)], [("Y", tbh, cg)])
                    pg.barrier()
                    wcnt = 0
                    dcnt = 0
                    for ex in range(NE):
                        bs_ = ex % 2
                        bcast_load(BC[bs_], cwT_d[ex:ex + 1, tsl_h], ("BC", bs_))
                        r0 = (l * NE + ex) * D
                        for fp in range(4):
                            w_ = wcnt % 2
                            wcnt += 1
                            dma(WG[w_], kview(eg[r0:r0 + D, fp * 256:(fp + 1) * 256], 16), [], [("WG", w_)], cast=True)
                            dma(WU[w_], kview(eu[r0:r0 + D, fp * 256:(fp + 1) * 256], 16), [], [("WU", w_)], cast=True)
                            for fj in range(2):
                                fc = fp * 2 + fj
                                fsl = slice(fj * 128, (fj + 1) * 128)
                                for tg2 in range(2):
                                    tsl = slice(tg2 * 512, (tg2 + 1) * 512)
                                    bg_, bu_ = bank(), bank()
                                    mm([(PS[bg_][:], [(WG[w_][:, kc, fsl], X1H[:, kc, tsl]) for kc in range(KC)])], [("WG", w_)] + x1h_ids, ("ps", bg_))
                                    mm([(PS[bu_][:], [(WU[w_][:, kc, fsl], X1H[:, kc, tsl]) for kc in range(KC)])], [("WU", w_)] + x1h_ids, ("ps", bu_))
                                    s_ = cnt % 2
                                    cnt += 1
                                    T1, T2 = TT[s_]
                                    act(T1, PS[bg_][:], AF.Silu, [("ps", bg_)], [("T1", s_)])
                                    tt(T2, T1, PS[bu_][:], ALU.mult, [("T1", s_), ("ps", bu_)], [("T2", s_)])
                                    tt(HS[:, fc, tsl], T2, BC[bs_][:, tsl], ALU.mult, [("T2", s_), ("BC", bs_)], [("HS", fc, tg2)])
                        r1 = (l * NE + ex) * DE
                        for cg in range(4):
                            d_ = dcnt % 2
                            dcnt += 1
                            dma(WD[d_], kview(ed[r1:r1 + DE, cg * 512:(cg + 1) * 512], 8), [], [("WD", d_)], cast=True)
                            for tbh in range(8):
                                rows = slice(tbh * 128, (tbh + 1) * 128)
                                b = bank()
                                mm([(PS[b][:], [(HS[:, fc, rows], WD[d_][:, fc, :]) for fc in range(8)])], [("WD", d_)] + [("HS", fc, tbh // 4) for fc in range(8)], ("ps", b))
                                ysl = YACC[:, tbh, cg * 512:(cg + 1) * 512]
                                tt(ysl, ysl, PS[b][:], ALU.add, [("Y", tbh, cg), ("ps", b)], [("Y", tbh, cg)])
                    pg.barrier()
                    ar.reset()
                    G2 = ar.f32(2048)
                    B2 = ar.f32(2048)
                    X1R = [ar.f32(2048) for _ in range(2)]
                    X2B = [ar.bf16(2048) for _ in range(2)]
                    ST = ar.f32(24)
                    MV = ar.f32(2)
                    RS = ar.f32(1)
                    bcast_load(G2, l2g[l:l + 1, :], "G2")
                    bcast_load(B2, l2b[l:l + 1, :], "B2")
                    def ln2_load(tbh):
                        tb = hf * 8 + tbh
                        dma(X1R[tbh % 2], xb[tb * 128:(tb + 1) * 128, :], [], [("X1R", tbh % 2)])

                    def ln2_A(tbh):
                        tb = hf * 8 + tbh
                        rows = slice(tb * 128, (tb + 1) * 128)
                        s_ = tbh % 2
                        Y = YACC[:, tbh, :]
                        yid = ("Yr", tbh)
                        stt(Y, X1R[s_], ALPHA, Y, ALU.mult, ALU.add, [("X1R", s_)], [yid])
                        layer_norm(Y, yid, G2, B2, "G2", "B2", 2048, ST, MV, RS, "SM")
                        tt(Y, Y, B2, ALU.add, [yid, "B2"], [yid], eng="pool")
                        if last:
                            dma(out[e_i * S + tb * 128:e_i * S + (tb + 1) * 128, :], Y, [yid], [("out", tb)])
                        else:
                            dma(xa[rows, :], Y, [yid], [("xa", tb)])

                    def ln2_B(tbh):
                        tb = hf * 8 + tbh
                        rows = slice(tb * 128, (tb + 1) * 128)
                        s_ = tbh % 2
                        Y = YACC[:, tbh, :]
                        yid = ("Yr", tbh)
                        if not last:
                            for q4 in range(4):
                                b_ = bank()
                                tr([(PS[b_][:, j * 128:(j + 1) * 128], Y[:, (q4 * 4 + j) * 128:(q4 * 4 + j + 1) * 128]) for j in range(4)], [yid, "CST"], ("ps", b_))
                                cp(X2B[s_][:, q4 * 512:(q4 + 1) * 512], PS[b_][:], [("ps", b_)], [("X2B", s_, q4)], eng="act")
                            dma(xaT.rearrange("(k p) s -> p k s", p=128)[:, :, rows], X2B[s_].rearrange("p (k t) -> p k t", k=16), [("X2B", s_, q) for q in range(4)], [("xaT", tb)])

                    ln2_load(0)
                    for it in range(9):
                        if it + 1 < 8:
                            ln2_load(it + 1)
                        if it < 8:
                            ln2_A(it)
                        if it >= 1:
                            ln2_B(it - 1)
                    pg.barrier()

        pg.barrier()

        @block.tensor
        def _(e):
            pg.run("pe", e)

        @block.vector
        def _(e):
            pg.run("dve", e)

        @block.scalar
        def _(e):
            pg.run("act", e)

        @block.gpsimd
        def _(e):
            pg.run("pool", e)

        @block.sync
        def _(e):
            pg.run("sp", e)

    return nc


def make_consts():
    c = np.zeros((128, NCONST), np.float32)
    i = np.arange(128)
    c[:, 0:128] = np.eye(128, dtype=np.float32)
    c[:, 128:256] = (i[:, None] > i[None, :]).astype(np.float32)
    c[:, 256:384] = 1.0
    c[:, 384:512] = (i[:, None] <= i[None, :]).astype(np.float32)
    t = np.arange(512)
    for j in range(4):
        c[:, 512 + j * 512:512 + (j + 1) * 512] = ((t[None, :] - 128 * j) > i[:, None]).astype(np.float32)
    return c


def make_in_maps(inp, ncores, epc, depth=DEPTH):
    DEPTH = depth
    f = lambda a: np.ascontiguousarray(np.asarray(a, dtype=np.float32)[:depth]) if np.asarray(a).shape[0] == 4 and np.asarray(a).ndim >= 2 else np.ascontiguousarray(np.asarray(a, dtype=np.float32))
    x = f(inp["x"])
    p = f(inp["p"])
    shared = {
        "w_in": f(inp["w_in"]).reshape(DEPTH * D, INW),
        "w_out_a": f(inp["w_out_a"]).reshape(DEPTH * 1024, D),
        "w_out_b": f(inp["w_out_b"]).reshape(DEPTH * 1024, D),
        "w_o": f(inp["w_o"]).reshape(DEPTH * D, D),
        "exp_g": f(inp["exp_w_gate"]).reshape(DEPTH * NE * D, DE),
        "exp_u": f(inp["exp_w_up"]).reshape(DEPTH * NE * D, DE),
        "exp_d": f(inp["exp_w_down"]).reshape(DEPTH * NE * DE, D),
        "ple_g": f(inp["ple_w_gate"]).reshape(DEPTH * D, D),
        "ple_p": f(inp["ple_w_proj"]).reshape(DEPTH * 256, D),
        "router_w": f(inp["router_w"]),
        "router_bias": f(inp["router_bias"]).reshape(1, 16),
        "gmlp_ln_g": f(inp["gmlp_ln_g"]),
        "gmlp_ln_b": f(inp["gmlp_ln_b"]),
        "ln1_g": f(inp["ln1_g"]),
        "ln1_b": f(inp["ln1_b"]),
        "ln2_g": f(inp["ln2_g"]),
        "ln2_b": f(inp["ln2_b"]),
        "gmlp_bs": f(inp["gmlp_bs"]).reshape(DEPTH, 1024),
        "gmlp_wsT": np.ascontiguousarray(f(inp["gmlp_ws"]).transpose(0, 1, 3, 2)).reshape(DEPTH * 8 * 128, 128),
        "consts": make_consts(),
    }
    maps = []
    for c in range(ncores):
        bs = list(range(c * epc, (c + 1) * epc))
        m = dict(shared)
        m["x"] = np.ascontiguousarray(x[bs].reshape(epc * S, D))
        m["xT"] = np.ascontiguousarray(x[bs].transpose(0, 2, 1)).reshape(epc * D, S)
        m["pT"] = np.ascontiguousarray(p[:, bs].transpose(0, 1, 3, 2)).reshape(DEPTH * epc * 256, S)
        maps.append(m)
    return maps


def kernel(**inputs):
    nc = build(EPC, DEPTH)
    maps = make_in_maps(inputs, NCORES, EPC)
    res = run_bass_kernel_spmd(nc, maps, core_ids=list(range(NCORES)))
    outs = [np.asarray(r["out"]).reshape(EPC, S, D) for r in res.results]
    return np.concatenate(outs, axis=0).astype(np.float32)
```

```python
import numpy as np
import concourse.bass as bass
import concourse.mybir as mybir
from concourse.bass_utils import run_bass_kernel_spmd

F32 = mybir.dt.float32
BF16 = mybir.dt.bfloat16
AF = mybir.ActivationFunctionType
ALU = mybir.AluOpType
AX = mybir.AxisListType

S = 2048
D = 2048
KC = 16
INW = 9216
NE = 16
DE = 1024
DEPTH = 4
ALPHA = float((2 * DEPTH) ** 0.25)
EPS = 1e-5
SCALE = float(128 ** -0.5)
NCORES = 8
EPC = 1
NCONST = 128 * 4 + 4 * 512


class Prog:
    def __init__(self, esem, dsems):
        self.q = {n: [] for n in ("pe", "dve", "act", "pool", "sp")}
        self.seen = {n: {} for n in self.q}
        self.esem = esem
        self.ecount = {}
        self.epoch = 0
        self.dsems = dsems
        self.dcount = {}
        self.drr = {n: 0 for n in dsems}
        self.lastw = {}
        self.readers = {}

    def semh(self, k):
        if isinstance(k, tuple):
            return self.dsems[k[0]][k[1]]
        return self.esem[k]

    def op(self, eng, fn, reads=(), writes=(), dma=False):
        seen = self.seen[eng]
        waits = []

        def need(dep):
            if dep is None:
                return
            k, v = dep
            if eng == "pe" and isinstance(k, str) and k.startswith("pe#"):
                return
            if seen.get(k, 0) >= v:
                return
            seen[k] = v
            waits.append((k, v))

        for r in reads:
            need(self.lastw.get(r))
        for w in writes:
            need(self.lastw.get(w))
            for k, v in self.readers.get(w, {}).items():
                need((k, v))
        if dma:
            lst = self.dsems[eng]
            i = self.drr[eng]
            self.drr[eng] = (i + 1) % len(lst)
            k = (eng, i)
            prev = self.dcount.get(k, 0)
            if prev:
                need((k, prev))
            self.dcount[k] = prev + 16
            comp = (k, prev + 16)
        else:
            ek = eng + "#" + str(self.epoch)
            self.ecount[ek] = self.ecount.get(ek, 0) + 1
            comp = (ek, self.ecount[ek])
        for r in reads:
            d = self.readers.setdefault(r, {})
            d[comp[0]] = max(d.get(comp[0], 0), comp[1])
        for w in writes:
            self.lastw[w] = comp
            self.readers[w] = {}
        self.q[eng].append((waits, fn, comp))

    def barrier(self):
        allk = [(k, v) for k, v in self.ecount.items() if v > 0]
        allk += [(k, v) for k, v in self.dcount.items() if v > 0]
        for eng in self.q:
            seen = self.seen[eng]
            waits = []
            for k, v in allk:
                if isinstance(k, str) and k.split("#")[0] == eng:
                    continue
                if seen.get(k, 0) >= v:
                    continue
                seen[k] = v
                waits.append((k, v))
            if waits:
                self.q[eng].append((waits, None, None))
        self.lastw.clear()
        self.readers.clear()

    def run(self, eng, e):
        for waits, fn, comp in self.q[eng]:
            for k, v in waits:
                e.wait_ge(self.semh(k), v)
            if fn is None:
                continue
            ins = fn(e)
            k, v = comp
            ins.then_inc(self.semh(k), 16 if isinstance(k, tuple) else 1)


def build(epc, depth_run, wd=DEPTH):
    nc = bass.Bass("TRN2", target_bir_lowering=False)
    dt = nc.dram_tensor

    def ein(name, shape):
        return dt(name, shape, F32, kind="ExternalInput").ap()

    x_in = ein("x", [epc * S, D])
    xT_in = ein("xT", [epc * D, S])
    pT_in = ein("pT", [wd * epc * 256, S])
    w_in = ein("w_in", [wd * D, INW])
    w_oa = ein("w_out_a", [wd * 1024, D])
    w_ob = ein("w_out_b", [wd * 1024, D])
    w_o = ein("w_o", [wd * D, D])
    eg = ein("exp_g", [wd * NE * D, DE])
    eu = ein("exp_u", [wd * NE * D, DE])
    ed = ein("exp_d", [wd * NE * DE, D])
    plg = ein("ple_g", [wd * D, D])
    plp = ein("ple_p", [wd * 256, D])
    rw = ein("router_w", [D, 16])
    rb = ein("router_bias", [1, 16])
    gg = ein("gmlp_ln_g", [wd, 1024])
    gb = ein("gmlp_ln_b", [wd, 1024])
    l1g = ein("ln1_g", [wd, D])
    l1b = ein("ln1_b", [wd, D])
    l2g = ein("ln2_g", [wd, D])
    l2b = ein("ln2_b", [wd, D])
    bsd = ein("gmlp_bs", [wd, 1024])
    wsT = ein("gmlp_wsT", [wd * 8 * 128, 128])
    cst = ein("consts", [128, NCONST])
    out = dt("out", [epc * S, D], F32, kind="ExternalOutput").ap()
    xa = dt("xa", [S, D], F32).ap()
    xb = dt("xb", [S, D], F32).ap()
    xaT = dt("xaT", [D, S], BF16).ap()
    x1T = dt("x1T", [D, S], BF16).ap()
    mixT = dt("mixT", [D, S], BF16).ap()
    cwT_d = dt("cwT_d", [16, S], F32).ap()

    import contextlib
    with contextlib.ExitStack() as st:
        R1t = st.enter_context(nc.sbuf_tensor("R1", [128, 32768], BF16))
        R2t = st.enter_context(nc.sbuf_tensor("R2", [128, 32768], BF16))
        ARt = st.enter_context(nc.sbuf_tensor("AR", [128, 15360], F32))
        CSTt = st.enter_context(nc.sbuf_tensor("CST", [128, NCONST], F32))
        CSTBt = st.enter_context(nc.sbuf_tensor("CSTB", [128, 256], BF16))
        PS = [st.enter_context(nc.psum_tensor(f"ps{i}", [128, 512], F32)) for i in range(8)]
        esem = {f"{n}#{ep}": st.enter_context(nc.semaphore(f"s_{n}_{ep}")) for n in ("pe", "dve", "act", "pool") for ep in range(epc)}
        dsems = {
            "sp": [st.enter_context(nc.semaphore(f"d_sp{i}")) for i in range(8)],
            "pool": [st.enter_context(nc.semaphore(f"d_pl{i}")) for i in range(8)],
        }
        block = st.enter_context(nc.Block())
        pg = Prog(esem, dsems)

        R1f = R1t[:]
        R2f = R2t[:]
        XT = R1f.rearrange("p (k s) -> p k s", k=16)
        R2v = R2f.rearrange("p (k s) -> p k s", k=16)
        IDENT = CSTt[:, 0:128]
        TRI = CSTt[:, 128:256]
        ONES = CSTt[:, 256:384]
        TRIU = CSTt[:, 384:512]
        MASKD = [CSTt[:, 512 + j * 512:512 + (j + 1) * 512] for j in range(4)]

        class Arena:
            def __init__(self):
                self.off = 0

            def reset(self):
                self.off = 0

            def f32(self, n):
                a = ARt[:, self.off:self.off + n]
                self.off += n
                assert self.off <= 15360, self.off
                return a

            def bf16(self, n):
                assert n % 2 == 0
                a = ARt[:, self.off:self.off + n // 2].bitcast(BF16)
                self.off += n // 2
                assert self.off <= 15360, self.off
                return a

        ar = Arena()
        uid = [0]

        def nid(tag):
            uid[0] += 1
            return (tag, uid[0])

        def mm(groups, reads, wid):
            def fn(e, groups=groups):
                ins = None
                for o, pairs in groups:
                    n = len(pairs)
                    for i, (l, r) in enumerate(pairs):
                        ins = e.matmul(o, l, r, start=(i == 0), stop=(i == n - 1))
                return ins
            pg.op("pe", fn, reads=reads, writes=[wid])

        def tr(groups, reads, wid):
            def fn(e, groups=groups):
                ins = None
                for o, i_ in groups:
                    ins = e.transpose(o, i_, IDENT)
                return ins
            pg.op("pe", fn, reads=reads, writes=[wid])

        def act(o, i_, func, reads, writes, scale=1.0, bias=0.0):
            def fn(e, o=o, i_=i_):
                return e.activation(out=o, in_=i_, func=func, bias=bias, scale=scale)
            pg.op("act", fn, reads=reads, writes=writes)

        def tt(o, a, b, op, reads, writes, eng="dve"):
            def fn(e, o=o, a=a, b=b):
                return e.tensor_tensor(out=o, in0=a, in1=b, op=op)
            pg.op(eng, fn, reads=reads, writes=writes)

        def ts(o, a, s1, s2, op0, op1, reads, writes):
            def fn(e, o=o, a=a):
                if s2 is None:
                    return e.tensor_scalar(out=o, in0=a, scalar1=s1, scalar2=None, op0=op0)
                return e.tensor_scalar(out=o, in0=a, scalar1=s1, scalar2=s2, op0=op0, op1=op1)
            pg.op("dve", fn, reads=reads, writes=writes)

        def stt(o, a, sc, b, op0, op1, reads, writes):
            def fn(e, o=o, a=a, b=b):
                return e.scalar_tensor_tensor(out=o, in0=a, scalar=sc, in1=b, op0=op0, op1=op1)
            pg.op("dve", fn, reads=reads, writes=writes)

        def cp(o, i_, reads, writes, eng="dve"):
            if eng == "act":
                def fn(e, o=o, i_=i_):
                    return e.copy(out=o, in_=i_)
            else:
                def fn(e, o=o, i_=i_):
                    return e.tensor_copy(out=o, in_=i_)
            pg.op(eng, fn, reads=reads, writes=writes)

        def red(o, i_, op, reads, writes):
            def fn(e, o=o, i_=i_):
                return e.tensor_reduce(out=o, in_=i_, axis=AX.X, op=op)
            pg.op("dve", fn, reads=reads, writes=writes)

        def dma(o, i_, reads, writes, cast=False):
            eng = "pool" if cast else "sp"

            def fn(e, o=o, i_=i_):
                return e.dma_start(out=o, in_=i_)
            pg.op(eng, fn, reads=reads, writes=writes, dma=True)

        def bcast_load(dst, src_row, wid):
            dma(dst, src_row.partition_broadcast(128)[:, 0, :], [], [wid])

        def kview(ap2d, kc):
            return ap2d.rearrange("(k p) n -> p k n", p=128)

        psr = [0]

        def bank(lo=0, hi=8):
            b = lo + psr[0] % (hi - lo)
            psr[0] += 1
            return b

        def gelu_a(ps_ap, psid, T1, t1id):
            act(T1, ps_ap, AF.Square, [psid], [t1id])
            ts(T1, T1, 0.044715, 1.0, ALU.mult, ALU.add, [t1id], [t1id])
            tt(T1, T1, ps_ap, ALU.mult, [t1id, psid], [t1id])

        def gelu_b(ps_ap, psid, dst, dstid, T1, T2, t1id, t2id):
            act(T2, T1, AF.Sigmoid, [t1id], [t2id], scale=1.5957691216057308)
            tt(dst, T2, ps_ap, ALU.mult, [t2id, psid], [dstid])

        def gelu(ps_ap, psid, dst, dstid, T1, T2, t1id, t2id):
            gelu_a(ps_ap, psid, T1, t1id)
            gelu_b(ps_ap, psid, dst, dstid, T1, T2, t1id, t2id)

        def layer_norm(T, tid, G, B, gid, bid, n, ST, MV, RS, smid):
            nch = n // 512
            for c in range(nch):
                def fn(e, c=c):
                    return e.bn_stats(out=ST[:, c * 6:(c + 1) * 6], in_=T[:, c * 512:(c + 1) * 512])
                pg.op("dve", fn, reads=[tid], writes=[smid])
            def fn2(e):
                return e.bn_aggr(out=MV, in_=ST[:, 0:nch * 6])
            pg.op("dve", fn2, reads=[smid], writes=[smid])
            act(RS, MV[:, 1:2], AF.Sqrt, [smid], ["RSQ"], bias=EPS)
            def fn3(e):
                return e.reciprocal(out=RS, in_=RS)
            pg.op("dve", fn3, reads=["RSQ"], writes=[smid])
            ts(MV[:, 1:2], MV[:, 0:1], RS, -1.0, ALU.mult, ALU.mult, [smid], [smid])
            def fn4(e):
                return e.activation(out=T, in_=T, func=AF.Identity, bias=MV[:, 1:2], scale=RS)
            pg.op("act", fn4, reads=[tid, smid], writes=[tid])
            tt(T, T, G, ALU.mult, [tid, gid], [tid], eng="pool")

        dma(CSTt[:], cst[:, :], [], ["CST"])
        cp(CSTBt[:, 0:256], CSTt[:, 128:384], ["CST"], ["CSTB"])
        TRIB = CSTBt[:, 0:128]
        ONESB = CSTBt[:, 128:256]
        pg.barrier()

        for e_i in range(epc):
            pg.epoch = e_i
            for l in range(depth_run):
                last = (l == depth_run - 1)
                wl = w_in[l * D:(l + 1) * D, :]
                for kc in range(KC):
                    if l == 0:
                        dma(XT[:, kc, :], xT_in[e_i * D + kc * 128:e_i * D + (kc + 1) * 128, :], [], [("XT", kc)], cast=True)
                    else:
                        dma(XT[:, kc, :], xaT[kc * 128:(kc + 1) * 128, :], [], [("XT", kc)])
                xt_ids = [("XT", kc) for kc in range(KC)]
                ar.reset()
                WS = [ar.bf16(16 * 512).rearrange("p (k n) -> p k n", k=16) for _ in range(2)]
                TT = [[ar.f32(512), ar.f32(512)] for _ in range(2)]
                for grp in range(2):
                    dma(WS[grp], kview(wl[:, grp * 512:(grp + 1) * 512], 16), [], [("WS", grp)], cast=True)
                pend = None
                cnt = 0
                for grp in range(2):
                    wsid = ("WS", grp)
                    for cc in range(4):
                        h = grp * 4 + cc
                        for tg in range(4):
                            b = bank()
                            mm([(PS[b][:], [(WS[grp][:, kc, cc * 128:(cc + 1) * 128], XT[:, kc, tg * 512:(tg + 1) * 512]) for kc in range(KC)])],
                               [wsid] + xt_ids, ("ps", b))
                            s_ = cnt % 2
                            cnt += 1
                            gelu_a(PS[b][:], ("ps", b), TT[s_][0], ("T1", s_))
                            if pend is not None:
                                gelu_b(*pend)
                            pend = (PS[b][:], ("ps", b), R2v[:, h, tg * 512:(tg + 1) * 512], ("R2", h, tg),
                                    TT[s_][0], TT[s_][1], ("T1", s_), ("T2", s_))
                gelu_b(*pend)
                pg.barrier()
                ar.reset()
                VW = R2f[:, 16384:32768].rearrange("p (k n) -> p k n", k=16)
                for hf in range(2):
                    dma(VW[:, :, hf * 512:(hf + 1) * 512], kview(wl[:, 1024 + hf * 512:1024 + (hf + 1) * 512], 16), [], [("VW", hf)], cast=True)
                GG = ar.f32(1024)
                GB_ = ar.f32(1024)
                BSB = ar.f32(1024)
                WSF = ar.f32(1024)
                WSB = ar.bf16(1024)
                VG = [ar.f32(1024) for _ in range(2)]
                VC = [ar.bf16(1024) for _ in range(3)]
                T1s = [[ar.f32(512), ar.f32(512)] for _ in range(2)]
                FB = [ar.f32(512) for _ in range(2)]
                ST = ar.f32(24)
                MV = ar.f32(2)
                RS = ar.f32(1)
                bcast_load(GG, gg[l:l + 1, :], "GG")
                bcast_load(GB_, gb[l:l + 1, :], "GB")
                bcast_load(BSB, bsd[l:l + 1, :], "BSB")
                dma(WSF.rearrange("p (h t) -> p h t", h=8), wsT[l * 1024:(l + 1) * 1024, :].rearrange("(h p) t -> p h t", p=128), [], ["WSF"])
                for h in range(8):
                    tt(WSB[:, h * 128:(h + 1) * 128], WSF[:, h * 128:(h + 1) * 128], TRIU, ALU.mult, ["WSF", "CST"], [("WSB", h)])
                wsb_ids = [("WSB", h) for h in range(8)]
                cnt = [0]

                def s1b_A(c):
                    s_ = c % 2
                    vgid = ("VG", s_)
                    for hf in range(2):
                        b = bank()
                        mm([(PS[b][:], [(XT[:, kc, c * 128:(c + 1) * 128], VW[:, kc, hf * 512:(hf + 1) * 512]) for kc in range(KC)])],
                           [("VW", hf)] + xt_ids, ("ps", b))
                        t_ = cnt[0] % 2
                        cnt[0] += 1
                        gelu(PS[b][:], ("ps", b), VG[s_][:, hf * 512:(hf + 1) * 512], vgid,
                             T1s[t_][0], T1s[t_][1], ("T1", t_), ("T2", t_))
                    layer_norm(VG[s_], vgid, GG, GB_, "GG", "GB", 1024, ST, MV, RS, "SM")
                    tt(VC[c % 3], VG[s_], GB_, ALU.add, [vgid, "GB"], [("VC", c % 3)], eng="pool")

                def s1b_B(c):
                    s_ = c % 3
                    for hh in range(2):
                        b = bank()
                        mm([(PS[b][:, j * 128:(j + 1) * 128], [(VC[s_][:, (hh * 4 + j) * 128:(hh * 4 + j + 1) * 128], WSB[:, (hh * 4 + j) * 128:(hh * 4 + j + 1) * 128])]) for j in range(4)],
                           [("VC", s_)] + wsb_ids, ("ps", b))
                        f_ = hh
                        F1 = FB[f_]
                        tt(F1, PS[b][:], BSB[:, hh * 512:(hh + 1) * 512], ALU.add, [("ps", b), "BSB"], [("F1", f_)])
                        dst = R2v[:, hh * 4:(hh + 1) * 4, c * 128:(c + 1) * 128]
                        ids = [("R2", hh * 4 + j, c // 4) for j in range(4)]
                        def fn(e, dst=dst, F1=F1):
                            return e.tensor_tensor(out=dst, in0=F1.rearrange("p (h t) -> p h t", h=4), in1=dst, op=ALU.mult)
                        pg.op("dve", fn, reads=[("F1", f_)] + ids, writes=ids)

                for it in range(18):
                    if it < 16:
                        s1b_A(it)
                    if it >= 2:
                        s1b_B(it - 2)
                pg.barrier()
                ar.reset()
                WQ = ar.bf16(16 * 128).rearrange("p (k n) -> p k n", k=16)
                WK = ar.bf16(16 * 128).rearrange("p (k n) -> p k n", k=16)
                WVh = ar.bf16(16 * 128).rearrange("p (k n) -> p k n", k=16)
                QT = ar.bf16(2048)
                KT = ar.bf16(2048)
                VH = ar.bf16(2048)
                E_ = [ar.f32(512) for _ in range(2)]
                SP = [ar.bf16(512) for _ in range(3)]
                LA = [ar.f32(512) for _ in range(2)]
                A_ = [ar.bf16(512) for _ in range(3)]
                for h in range(8):
                    dma(WQ, kview(wl[:, 2048 + h * 128:2048 + (h + 1) * 128], 16), [], ["WQ"], cast=True)
                    dma(WK, kview(wl[:, 3072 + h * 128:3072 + (h + 1) * 128], 16), [], ["WK"], cast=True)
                    dma(WVh, kview(wl[:, 4096 + h * 128:4096 + (h + 1) * 128], 16), [], ["WV"], cast=True)
                    for tg in range(4):
                        b = bank(0, 8)
                        mm([(PS[b][:], [(WQ[:, kc, :], XT[:, kc, tg * 512:(tg + 1) * 512]) for kc in range(KC)])], ["WQ"] + xt_ids, ("ps", b))
                        cp(QT[:, tg * 512:(tg + 1) * 512], PS[b][:], [("ps", b)], [("QT", tg)], eng="act")
                        b = bank(0, 8)
                        mm([(PS[b][:], [(WK[:, kc, :], XT[:, kc, tg * 512:(tg + 1) * 512]) for kc in range(KC)])], ["WK"] + xt_ids, ("ps", b))
                        cp(KT[:, tg * 512:(tg + 1) * 512], PS[b][:], [("ps", b)], [("KT", tg)])
                        b = bank(0, 8)
                        mm([(PS[b][:, j * 128:(j + 1) * 128], [(XT[:, kc, (tg * 4 + j) * 128:(tg * 4 + j + 1) * 128], WVh[:, kc, :]) for kc in range(KC)]) for j in range(4)],
                           ["WV"] + xt_ids, ("ps", b))
                        cp(VH[:, tg * 512:(tg + 1) * 512], PS[b][:], [("ps", b)], [("VH", tg)], eng="act")
                    for g in range(4):
                        t0 = g * 512
                        ob = 6 + g % 2
                        tiles = list(range(4 * g + 3, -1, -1))
                        n = len(tiles)

                        def stZ(i, g=g, t0=t0, tiles=tiles):
                            kc = tiles[i]
                            zb = 1 + i % 3
                            mm([(PS[zb][:], [(KT[:, kc * 128:(kc + 1) * 128], QT[:, t0:t0 + 512])])], [("KT", kc // 4), ("QT", g)], ("ps", zb))

                        def stA(i, g=g, tiles=tiles):
                            kc = tiles[i]
                            zb = 1 + i % 3
                            e_, sp_ = i % 2, i % 3
                            diag = kc >= 4 * g
                            j = kc - 4 * g
                            act(E_[e_], PS[zb][:], AF.Exp, [("ps", zb)], [("E", e_)], scale=SCALE)
                            act(SP[sp_], E_[e_], AF.Ln, [("E", e_)], [("SP", sp_)], bias=1.0)
                            if diag:
                                tt(SP[sp_], SP[sp_], MASKD[j], ALU.mult, [("SP", sp_), "CST"], [("SP", sp_)])

                        def stB(i, g=g, tiles=tiles):
                            kc = tiles[i]
                            zb = 1 + i % 3
                            sb = 4 + i % 2
                            sp_, la_, a_ = i % 3, i % 2, i % 3
                            diag = kc >= 4 * g
                            j = kc - 4 * g
                            rb = 0
                            if i > 0:
                                def fnr_(e, rb=rb, i=i):
                                    return e.matmul(PS[rb][:], ONESB, SP[(i - 1) % 3], start=(i == 1), stop=True, skip_group_check=True)
                                pg.op("pe", fnr_, reads=[("SP", (i - 1) % 3), "CSTB"], writes=[("ps", rb)])
                            mm([(PS[sb][:], [(TRIB, SP[sp_])])], [("SP", sp_), "CSTB"], ("ps", sb))
                            stt(LA[la_], PS[zb][:], SCALE, SP[sp_], ALU.mult, ALU.subtract, [("ps", zb), ("SP", sp_)], [("LA", la_)])
                            tt(LA[la_], LA[la_], PS[sb][:], ALU.subtract, [("LA", la_), ("ps", sb)], [("LA", la_)])
                            if i > 0:
                                tt(LA[la_], LA[la_], PS[rb][:], ALU.subtract, [("LA", la_), ("ps", rb)], [("LA", la_)])
                            act(A_[a_], LA[la_], AF.Exp, [("LA", la_)], [("A", a_)])
                            if diag:
                                tt(A_[a_], A_[a_], MASKD[j], ALU.mult, [("A", a_), "CST"], [("A", a_)])

                        def stC(i, ob=ob, tiles=tiles, n=n):
                            kc = tiles[i]
                            a_ = i % 3
                            def fn(e, ob=ob, kc=kc, a_=a_, i=i, n=n):
                                return e.matmul(PS[ob][:], VH[:, kc * 128:(kc + 1) * 128], A_[a_], start=(i == 0), stop=(i == n - 1), skip_group_check=True)
                            pg.op("pe", fn, reads=[("VH", kc // 4), ("A", a_)], writes=[("ps", ob)])

                        stZ(0)
                        for it in range(n + 2):
                            if it < n:
                                stA(it)
                            if 0 <= it - 1 < n:
                                stB(it - 1)
                            if 0 <= it - 2 < n:
                                stC(it - 2)
                            if it + 1 < n:
                                stZ(it + 1)
                        cp(R2v[:, 8 + h, t0:t0 + 512], PS[ob][:], [("ps", ob)], [("R2", 8 + h, g)], eng="act")
                pg.barrier()
                ar.reset()
                WGA = [ar.bf16(16 * 256).rearrange("p (k n) -> p k n", k=16) for _ in range(2)]
                WGB = [ar.bf16(16 * 256).rearrange("p (k n) -> p k n", k=16) for _ in range(2)]
                WA = [ar.bf16(8 * 256).rearrange("p (k n) -> p k n", k=8) for _ in range(2)]
                WB = [ar.bf16(8 * 256).rearrange("p (k n) -> p k n", k=8) for _ in range(2)]
                TT = [[ar.f32(512), ar.f32(512)] for _ in range(2)]
                MST = [ar.bf16(2048) for _ in range(1)]
                cnt = 0
                for cgp in range(8):
                    w_ = cgp % 2
                    c0 = cgp * 256
                    dma(WGA[w_], kview(wl[:, 5120 + c0:5120 + c0 + 256], 16), [], [("WGA", w_)], cast=True)
                    dma(WA[w_], kview(w_oa[l * 1024:(l + 1) * 1024, c0:c0 + 256], 8), [], [("WA", w_)], cast=True)
                    dma(WGB[w_], kview(wl[:, 7168 + c0:7168 + c0 + 256], 16), [], [("WGB", w_)], cast=True)
                    dma(WB[w_], kview(w_ob[l * 1024:(l + 1) * 1024, c0:c0 + 256], 8), [], [("WB", w_)], cast=True)
                    for cc in range(2):
                        c = cgp * 2 + cc
                        m_ = 0
                        for tg in range(4):
                            tsl = slice(tg * 512, (tg + 1) * 512)
                            csl = slice(cc * 128, (cc + 1) * 128)
                            bga, bya, bgb, byb = bank(), bank(), bank(), bank()
                            mm([(PS[bga][:], [(WGA[w_][:, kc, csl], XT[:, kc, tsl]) for kc in range(16)])], [("WGA", w_)] + xt_ids, ("ps", bga))
                            mm([(PS[bya][:], [(WA[w_][:, kc, csl], R2v[:, kc, tsl]) for kc in range(8)])], [("WA", w_)] + [("R2", kc, tg) for kc in range(8)], ("ps", bya))
                            mm([(PS[bgb][:], [(WGB[w_][:, kc, csl], XT[:, kc, tsl]) for kc in range(16)])], [("WGB", w_)] + xt_ids, ("ps", bgb))
                            mm([(PS[byb][:], [(WB[w_][:, kc, csl], R2v[:, 8 + kc, tsl]) for kc in range(8)])], [("WB", w_)] + [("R2", 8 + kc, tg) for kc in range(8)], ("ps", byb))
                            s_ = cnt % 2
                            cnt += 1
                            T1, T2 = TT[s_]
                            act(T1, PS[bga][:], AF.Sigmoid, [("ps", bga)], [("T1", s_)])
                            act(T2, PS[bgb][:], AF.Sigmoid, [("ps", bgb)], [("T2", s_)])
                            tt(T1, T1, PS[bya][:], ALU.mult, [("T1", s_), ("ps", bya)], [("T1", s_)])
                            tt(T2, T2, PS[byb][:], ALU.mult, [("T2", s_), ("ps", byb)], [("T2", s_)])
                            tt(MST[m_][:, tsl], T1, T2, ALU.add, [("T1", s_), ("T2", s_)], [("MST", m_, tg)])
                        dma(mixT[c * 128:(c + 1) * 128, :], MST[m_], [("MST", m_, tg) for tg in range(4)], [("mixT", c)])
                pg.barrier()
                ar.reset()
                WO = XT
                for kc in range(KC):
                    dma(WO[:, kc, :], w_o[l * D + kc * 128:l * D + (kc + 1) * 128, :], [], [("WO", kc)], cast=True)
                    dma(R2v[:, kc, :], mixT[kc * 128:(kc + 1) * 128, :], [], [("MX", kc)])
                wo_ids = [("WO", kc) for kc in range(KC)]
                mx_ids = [("MX", kc) for kc in range(KC)]
                Tb = [ar.f32(2048) for _ in range(3)]
                G1 = ar.f32(2048)
                B1 = ar.f32(2048)
                X1F = ar.f32(2048)
                X1B = ar.bf16(2048)
                RW = ar.f32(256)
                RB = ar.f32(16)
                CWT = [ar.f32(128) for _ in range(2)]
                ST = ar.f32(24)
                MV = ar.f32(2)
                RS = ar.f32(1)
                sm = [ar.f32(16) for _ in range(8)]
                AFF, SEL, EQ, SEL2, EM, W_, CW, _u = sm
                CWs = [CW, _u]
                M1 = ar.f32(4)
                M2 = ar.f32(4)
                GS = ar.f32(4)
                GMK = ar.f32(4)
                GM = ar.f32(1)
                WSUM = ar.f32(1)
                RWS = ar.f32(1)
                bcast_load(G1, l1g[l:l + 1, :], "G1")
                bcast_load(B1, l1b[l:l + 1, :], "B1")
                bcast_load(RB, rb[0:1, :], "RB")
                dma(RW.rearrange("p (k e) -> p k e", k=16), rw.rearrange("(k p) e -> p k e", p=128), [], ["RW"])

                def s4_load(tb):
                    T = Tb[tb % 3]
                    if l == 0:
                        dma(T, x_in[e_i * S + tb * 128:e_i * S + (tb + 1) * 128, :], [], [("T", tb % 3)])
                    else:
                        dma(T, xa[tb * 128:(tb + 1) * 128, :], [], [("T", tb % 3)])

                def s4_A(tb):
                    T = Tb[tb % 3]
                    tid = ("T", tb % 3)
                    rows = slice(tb * 128, (tb + 1) * 128)
                    for cg in range(4):
                        mm([(PS[cg][:], [(R2v[:, kc, rows], WO[:, kc, cg * 512:(cg + 1) * 512]) for kc in range(KC)])], mx_ids + wo_ids, ("ps", cg))
                        stt(T[:, cg * 512:(cg + 1) * 512], T[:, cg * 512:(cg + 1) * 512], ALPHA, PS[cg][:], ALU.mult, ALU.add, [tid, ("ps", cg)], [tid])
                    layer_norm(T, tid, G1, B1, "G1", "B1", 2048, ST, MV, RS, "SM")
                    tt(T, T, B1, ALU.add, [tid, "B1"], [tid], eng="pool")
                    dma(xb[rows, :], T, [tid], [("xb", tb)])

                def s4_B(tb):
                    T = Tb[tb % 3]
                    tid = ("T", tb % 3)
                    rows = slice(tb * 128, (tb + 1) * 128)
                    for q4 in range(4):
                        b_ = 4 + q4 % 2
                        tr([(PS[b_][:, j * 128:(j + 1) * 128], T[:, (q4 * 4 + j) * 128:(q4 * 4 + j + 1) * 128]) for j in range(4)], [tid, "CST"], ("ps", b_))
                        cp(X1F[:, q4 * 512:(q4 + 1) * 512], PS[b_][:], [("ps", b_)], [("X1F", q4)], eng="act")
                        cp(X1B[:, q4 * 512:(q4 + 1) * 512], X1F[:, q4 * 512:(q4 + 1) * 512], [("X1F", q4)], [("X1B", q4)])
                    dma(x1T.rearrange("(k p) s -> p k s", p=128)[:, :, rows], X1B.rearrange("p (k t) -> p k t", k=16), [("X1B", q) for q in range(4)], [("x1T", tb)])
                    X1Fv = X1F.rearrange("p (k t) -> p k t", k=16)
                    RWv = RW.rearrange("p (k e) -> p k e", k=16)
                    mm([(PS[6][:, 0:16], [(X1Fv[:, kc, :], RWv[:, kc, :]) for kc in range(KC)])], [("X1F", q) for q in range(4)] + ["RW"], ("ps", 6))
                    act(AFF, PS[6][:, 0:16], AF.Sigmoid, [("ps", 6)], ["AFF"])
                    tt(SEL, AFF, RB, ALU.add, ["AFF", "RB"], ["SEL"])
                    red(M1, SEL.rearrange("p (g e) -> p g e", g=4), ALU.max, ["SEL"], ["M1"])
                    for g_ in range(4):
                        ts(EQ[:, g_ * 4:(g_ + 1) * 4], SEL[:, g_ * 4:(g_ + 1) * 4], M1[:, g_:g_ + 1], None, ALU.is_equal, None, ["SEL", "M1"], ["EQ"])
                    stt(SEL2, EQ, -1.0e9, SEL, ALU.mult, ALU.add, ["EQ", "SEL"], ["SEL2"])
                    red(M2, SEL2.rearrange("p (g e) -> p g e", g=4), ALU.max, ["SEL2"], ["M2"])
                    tt(GS, M1, M2, ALU.add, ["M1", "M2"], ["GS"])
                    red(GM, GS, ALU.max, ["GS"], ["GM"])
                    ts(GMK, GS, GM[:, 0:1], None, ALU.is_equal, None, ["GS", "GM"], ["GMK"])
                    for g_ in range(4):
                        ts(EM[:, g_ * 4:(g_ + 1) * 4], SEL[:, g_ * 4:(g_ + 1) * 4], M2[:, g_:g_ + 1], GMK[:, g_:g_ + 1], ALU.is_ge, ALU.mult, ["SEL", "M2", "GMK"], ["EM"])
                    tt(W_, AFF, EM, ALU.mult, ["AFF", "EM"], ["W"])
                    red(WSUM, W_, ALU.add, ["W"], ["WSUM"])
                    def fnr(e):
                        return e.reciprocal(out=RWS, in_=WSUM)
                    pg.op("dve", fnr, reads=["WSUM"], writes=["RWS"])
                    ts(CWs[tb % 2], W_, RWS[:, 0:1], None, ALU.mult, None, ["W", "RWS"], [("CW", tb % 2)])

                def s4_C(tb):
                    rows = slice(tb * 128, (tb + 1) * 128)
                    c_ = tb % 2
                    tr([(PS[7][0:16, 0:128], CWs[c_])], [("CW", c_), "CST"], ("ps", 7))
                    cp(CWT[c_][0:16, :], PS[7][0:16, 0:128], [("ps", 7)], [("CWT", c_)], eng="act")
                    dma(cwT_d[:, rows], CWT[c_][0:16, :], [("CWT", c_)], [("cwT_d", tb)])

                s4_load(0)
                for it in range(18):
                    if it + 1 < 16:
                        s4_load(it + 1)
                    if it < 16:
                        s4_A(it)
                    if 1 <= it <= 16:
                        s4_B(it - 1)
                    if 2 <= it <= 17:
                        s4_C(it - 2)
                pg.barrier()
                YACC = R1f.bitcast(F32).rearrange("p (t n) -> p t n", t=8)
                X1H = R2f[:, 0:16384].rearrange("p (k s) -> p k s", k=16)
                HS = R2f[:, 16384:24576].rearrange("p (k s) -> p k s", k=8)
                WD = [R2f[:, 24576 + i * 4096:24576 + (i + 1) * 4096].rearrange("p (k n) -> p k n", k=8) for i in range(2)]
                for hf in range(2):
                    ar.reset()
                    WG = [ar.bf16(16 * 256).rearrange("p (k n) -> p k n", k=16) for _ in range(2)]
                    WU = [ar.bf16(16 * 256).rearrange("p (k n) -> p k n", k=16) for _ in range(2)]
                    BC = [ar.f32(1024) for _ in range(2)]
                    TT = [[ar.f32(512), ar.f32(512)] for _ in range(2)]
                    PTH = ar.bf16(2 * 1024).rearrange("p (k s) -> p k s", k=2)
                    PLP = ar.bf16(2 * 2048).rearrange("p (k n) -> p k n", k=2)
                    PLG = [ARt[:, i * 4096:(i + 1) * 4096].bitcast(BF16).rearrange("p (k n) -> p k n", k=16) for i in range(2)]
                    tsl_h = slice(hf * 1024, (hf + 1) * 1024)
                    for kc in range(KC):
                        dma(X1H[:, kc, :], x1T[kc * 128:(kc + 1) * 128, tsl_h], [], [("X1H", kc)])
                    x1h_ids = [("X1H", kc) for kc in range(KC)]
                    prow = (l * epc + e_i) * 256
                    dma(PTH, kview(pT_in[prow:prow + 256, tsl_h], 2), [], ["PTH"], cast=True)
                    dma(PLP, kview(plp[l * 256:(l + 1) * 256, :], 2), [], ["PLP"], cast=True)
                    cnt = 0
                    for cg in range(4):
                        pl_ = cg % 2
                        dma(PLG[pl_], kview(plg[l * D:(l + 1) * D, cg * 512:(cg + 1) * 512], 16), [], [("PLG", pl_)], cast=True)
                        for tbh in range(8):
                            rows = slice(tbh * 128, (tbh + 1) * 128)
                            ba, bb = bank(), bank()
                            mm([(PS[ba][:], [(X1H[:, kc, rows], PLG[pl_][:, kc, :]) for kc in range(KC)])], x1h_ids + [("PLG", pl_)], ("ps", ba))
                            mm([(PS[bb][:], [(PTH[:, kc, rows], PLP[:, kc, cg * 512:(cg + 1) * 512]) for kc in range(2)])], ["PTH", "PLP"], ("ps", bb))
                            s_ = cnt % 2
                            cnt += 1
                            T1 = TT[s_][0]
                            act(T1, PS[ba][:], AF.Sigmoid, [("ps", ba)], [("T1", s_)])
                            tt(YACC[:, tbh, cg * 512:(cg + 1) * 512], T1, PS[bb][:], ALU.mult, [("T1", s_), ("ps", bb)], [("Y", tbh, cg)])
                    pg.barrier()
                    wcnt = 0
                    dcnt = 0
                    for ex in range(NE):
                        bs_ = ex % 2
                        bcast_load(BC[bs_], cwT_d[ex:ex + 1, tsl_h], ("BC", bs_))
                        r0 = (l * NE + ex) * D
                        for fp in range(4):
                            w_ = wcnt % 2
                            wcnt += 1
                            dma(WG[w_], kview(eg[r0:r0 + D, fp * 256:(fp + 1) * 256], 16), [], [("WG", w_)], cast=True)
                            dma(WU[w_], kview(eu[r0:r0 + D, fp * 256:(fp + 1) * 256], 16), [], [("WU", w_)], cast=True)
                            for fj in range(2):
                                fc = fp * 2 + fj
                                fsl = slice(fj * 128, (fj + 1) * 128)
                                for tg2 in range(2):
                                    tsl = slice(tg2 * 512, (tg2 + 1) * 512)
                                    bg_, bu_ = bank(), bank()
                                    mm([(PS[bg_][:], [(WG[w_][:, kc, fsl], X1H[:, kc, tsl]) for kc in range(KC)])], [("WG", w_)] + x1h_ids, ("ps", bg_))
                                    mm([(PS[bu_][:], [(WU[w_][:, kc, fsl], X1H[:, kc, tsl]) for kc in range(KC)])], [("WU", w_)] + x1h_ids, ("ps", bu_))
                                    s_ = cnt % 2
                                    cnt += 1
                                    T1, T2 = TT[s_]
                                    act(T1, PS[bg_][:], AF.Silu, [("ps", bg_)], [("T1", s_)])
                                    tt(T2, T1, PS[bu_][:], ALU.mult, [("T1", s_), ("ps", bu_)], [("T2", s_)])
                                    tt(HS[:, fc, tsl], T2, BC[bs_][:, tsl], ALU.mult, [("T2", s_), ("BC", bs_)], [("HS", fc, tg2)])
                        r1 = (l * NE + ex) * DE
                        for cg in range(4):
                            d_ = dcnt % 2
                            dcnt += 1
                            dma(WD[d_], kview(ed[r1:r1 + DE, cg * 512:(cg + 1) * 512], 8), [], [("WD", d_)], cast=True)
                            for tbh in range(8):
                                rows = slice(tbh * 128, (tbh + 1) * 128)
                                b = bank()
                                mm([(PS[b][:], [(HS[:, fc, rows], WD[d_][:, fc, :]) for fc in range(8)])], [("WD", d_)] + [("HS", fc, tbh // 4) for fc in range(8)], ("ps", b))
                                ysl = YACC[:, tbh, cg * 512:(cg + 1) * 512]
                                tt(ysl, ysl, PS[b][:], ALU.add, [("Y", tbh, cg), ("ps", b)], [("Y", tbh, cg)])
                    pg.barrier()
                    ar.reset()
                    G2 = ar.f32(2048)
                    B2 = ar.f32(2048)
                    X1R = [ar.f32(2048) for _ in range(2)]
                    X2B = [ar.bf16(2048) for _ in range(2)]
                    ST = ar.f32(24)
                    MV = ar.f32(2)
                    RS = ar.f32(1)
                    bcast_load(G2, l2g[l:l + 1, :], "G2")
                    bcast_load(B2, l2b[l:l + 1, :], "B2")
                    def ln2_load(tbh):
                        tb = hf * 8 + tbh
                        dma(X1R[tbh % 2], xb[tb * 128:(tb + 1) * 128, :], [], [("X1R", tbh % 2)])

                    def ln2_A(tbh):
                        tb = hf * 8 + tbh
                        rows = slice(tb * 128, (tb + 1) * 128)
                        s_ = tbh % 2
                        Y = YACC[:, tbh, :]
                        yid = ("Yr", tbh)
                        stt(Y, X1R[s_], ALPHA, Y, ALU.mult, ALU.add, [("X1R", s_)], [yid])
                        layer_norm(Y, yid, G2, B2, "G2", "B2", 2048, ST, MV, RS, "SM")
                        tt(Y, Y, B2, ALU.add, [yid, "B2"], [yid], eng="pool")
                        if last:
                            dma(out[e_i * S + tb * 128:e_i * S + (tb + 1) * 128, :], Y, [yid], [("out", tb)])
                        else:
                            dma(xa[rows, :], Y, [yid], [("xa", tb)])

                    def ln2_B(tbh):
                        tb = hf * 8 + tbh
                        rows = slice(tb * 128, (tb + 1) * 128)
                        s_ = tbh % 2
                        Y = YACC[:, tbh, :]
                        yid = ("Yr", tbh)
                        if not last:
                            for q4 in range(4):
                                b_ = bank()
                                tr([(PS[b_][:, j * 128:(j + 1) * 128], Y[:, (q4 * 4 + j) * 128:(q4 * 4 + j + 1) * 128]) for j in range(4)], [yid, "CST"], ("ps", b_))
                                cp(X2B[s_][:, q4 * 512:(q4 + 1) * 512], PS[b_][:], [("ps", b_)], [("X2B", s_, q4)], eng="act")
                            dma(xaT.rearrange("(k p) s -> p k s", p=128)[:, :, rows], X2B[s_].rearrange("p (k t) -> p k t", k=16), [("X2B", s_, q) for q in range(4)], [("xaT", tb)])

                    ln2_load(0)
                    for it in range(9):
                        if it + 1 < 8:
                            ln2_load(it + 1)
                        if it < 8:
                            ln2_A(it)
                        if it >= 1:
                            ln2_B(it - 1)
                    pg.barrier()

        pg.barrier()

        @block.tensor
        def _(e):
            pg.run("pe", e)

        @block.vector
        def _(e):
            pg.run("dve", e)

        @block.scalar
        def _(e):
            pg.run("act", e)

        @block.gpsimd
        def _(e):
            pg.run("pool", e)

        @block.sync
        def _(e):
            pg.run("sp", e)

    return nc


def make_consts():
    c = np.zeros((128, NCONST), np.float32)
    i = np.arange(128)
    c[:, 0:128] = np.eye(128, dtype=np.float32)
    c[:, 128:256] = (i[:, None] > i[None, :]).astype(np.float32)
    c[:, 256:384] = 1.0
    c[:, 384:512] = (i[:, None] <= i[None, :]).astype(np.float32)
    t = np.arange(512)
    for j in range(4):
        c[:, 512 + j * 512:512 + (j + 1) * 512] = ((t[None, :] - 128 * j) > i[:, None]).astype(np.float32)
    return c


def make_in_maps(inp, ncores, epc, depth=DEPTH):
    DEPTH = depth
    f = lambda a: np.ascontiguousarray(np.asarray(a, dtype=np.float32)[:depth]) if np.asarray(a).shape[0] == 4 and np.asarray(a).ndim >= 2 else np.ascontiguousarray(np.asarray(a, dtype=np.float32))
    x = f(inp["x"])
    p = f(inp["p"])
    shared = {
        "w_in": f(inp["w_in"]).reshape(DEPTH * D, INW),
        "w_out_a": f(inp["w_out_a"]).reshape(DEPTH * 1024, D),
        "w_out_b": f(inp["w_out_b"]).reshape(DEPTH * 1024, D),
        "w_o": f(inp["w_o"]).reshape(DEPTH * D, D),
        "exp_g": f(inp["exp_w_gate"]).reshape(DEPTH * NE * D, DE),
        "exp_u": f(inp["exp_w_up"]).reshape(DEPTH * NE * D, DE),
        "exp_d": f(inp["exp_w_down"]).reshape(DEPTH * NE * DE, D),
        "ple_g": f(inp["ple_w_gate"]).reshape(DEPTH * D, D),
        "ple_p": f(inp["ple_w_proj"]).reshape(DEPTH * 256, D),
        "router_w": f(inp["router_w"]),
        "router_bias": f(inp["router_bias"]).reshape(1, 16),
        "gmlp_ln_g": f(inp["gmlp_ln_g"]),
        "gmlp_ln_b": f(inp["gmlp_ln_b"]),
        "ln1_g": f(inp["ln1_g"]),
        "ln1_b": f(inp["ln1_b"]),
        "ln2_g": f(inp["ln2_g"]),
        "ln2_b": f(inp["ln2_b"]),
        "gmlp_bs": f(inp["gmlp_bs"]).reshape(DEPTH, 1024),
        "gmlp_wsT": np.ascontiguousarray(f(inp["gmlp_ws"]).transpose(0, 1, 3, 2)).reshape(DEPTH * 8 * 128, 128),
        "consts": make_consts(),
    }
    maps = []
    for c in range(ncores):
        bs = list(range(c * epc, (c + 1) * epc))
        m = dict(shared)
        m["x"] = np.ascontiguousarray(x[bs].reshape(epc * S, D))
        m["xT"] = np.ascontiguousarray(x[bs].transpose(0, 2, 1)).reshape(epc * D, S)
        m["pT"] = np.ascontiguousarray(p[:, bs].transpose(0, 1, 3, 2)).reshape(DEPTH * epc * 256, S)
        maps.append(m)
    return maps


def kernel(**inputs):
    nc = build(EPC, DEPTH)
    maps = make_in_maps(inputs, NCORES, EPC)
    res = run_bass_kernel_spmd(nc, maps, core_ids=list(range(NCORES)))
    outs = [np.asarray(r["out"]).reshape(EPC, S, D) for r in res.results]
    return np.concatenate(outs, axis=0).astype(np.float32)
```
